# Optimizing a Trainium2 kernel written in Bass

```python
import jax
import jax.numpy as jnp
from jax import lax
import numpy as np

D_MODEL = 1024
BATCH = 4
SEQ = 8192
DEPTH = 2
DEC_BATCH = 16
DEC_SEQ = 2048
PAST_LEN = 128

GRID_W = 64
MLSTM_HEADS = 4
MLSTM_WIDTH = D_MODEL
MLSTM_HEAD_DIM = MLSTM_WIDTH // MLSTM_HEADS
MLSTM_CHUNK = 128
NA_HEADS = 16
NA_HEAD_DIM = 64
NA_WIDTH = NA_HEADS * NA_HEAD_DIM
NA_KH = 8
NA_KW = 16
LRU_WIDTH = D_MODEL
LRU_BLOCKS = 16
LRU_BLOCK = LRU_WIDTH // LRU_BLOCKS
LRU_C = 8.0
CONV_W = 4
N_BRANCH = 3
N_EXPERTS = 16
EC_FACTOR = 2
D_EXPERT = 2048
N_MOD = 6
EPS = 1e-6
NEG_INF = -1e30
IN_WIDTHS = (MLSTM_WIDTH, MLSTM_WIDTH, MLSTM_WIDTH, MLSTM_WIDTH, 4 * MLSTM_HEADS, NA_WIDTH, NA_WIDTH, NA_WIDTH, LRU_WIDTH, LRU_WIDTH, N_BRANCH * D_MODEL)
N_IN = sum(IN_WIDTHS)

kernel_name = 'hybrid_mlstm_natten_rglru_ec_encoder'


def _rmsnorm(x, w):
    xf = x.astype(jnp.float32)
    y = xf * lax.rsqrt(jnp.mean(xf * xf, axis=-1, keepdims=True) + EPS)
    return (y * w.astype(jnp.float32)).astype(x.dtype)


def _split_in(proj):
    points = []
    acc = 0
    for w in IN_WIDTHS[:-1]:
        acc += w
        points.append(acc)
    return jnp.split(proj, points, axis=-1)


def _mlstm_scan(q, k, v, ig, lf):
    B, H, T, dh = q.shape
    L = MLSTM_CHUNK
    nc = T // L

    def chunks(t):
        return jnp.moveaxis(t.reshape((B, H, nc, L) + t.shape[3:]), 2, 0)

    tril = jnp.tril(jnp.ones((L, L), dtype=bool))

    def step(carry, inp):
        C, n, m = carry
        qc, kc, vc, ic, fc = inp
        b = jnp.cumsum(fc, axis=-1)
        dmat = jnp.where(tril, b[..., :, None] - b[..., None, :] + ic[..., None, :], NEG_INF)
        g = b + m[..., None]
        m_row = jnp.maximum(g, jnp.max(dmat, axis=-1))
        w_intra = jnp.exp(dmat - m_row[..., None])
        w_inter = jnp.exp(g - m_row)
        s = jnp.einsum('bhid,bhjd->bhij', qc, kc) * w_intra
        num = jnp.einsum('bhij,bhje->bhie', s, vc) + w_inter[..., None] * jnp.einsum('bhid,bhde->bhie', qc, C)
        den = jnp.sum(s, axis=-1) + w_inter * jnp.einsum('bhid,bhd->bhi', qc, n)
        h = num / jnp.maximum(jnp.abs(den), jnp.exp(-m_row))[..., None]
        b_last = b[..., -1]
        w_last = b_last[..., None] - b + ic
        m_new = jnp.maximum(b_last + m, jnp.max(w_last, axis=-1))
        w_k = jnp.exp(w_last - m_new[..., None])
        decay = jnp.exp(b_last + m - m_new)
        C_new = decay[..., None, None] * C + jnp.einsum('bhj,bhjd,bhje->bhde', w_k, kc, vc)
        n_new = decay[..., None] * n + jnp.einsum('bhj,bhjd->bhd', w_k, kc)
        return (C_new, n_new, m_new), h

    init = (jnp.zeros((B, H, dh, dh), jnp.float32), jnp.zeros((B, H, dh), jnp.float32), jnp.zeros((B, H), jnp.float32))
    _, h = lax.scan(step, init, (chunks(q), chunks(k), chunks(v), chunks(ig), chunks(lf)))
    return jnp.moveaxis(h, 0, 2).reshape(B, H, T, dh)


def _mlstm_bidir(q, k, v, o, gates, norm_w):
    B, T, _ = q.shape
    H, dh = MLSTM_HEADS, MLSTM_HEAD_DIM

    def heads(t):
        return t.astype(jnp.float32).reshape(B, T, H, dh).transpose(0, 2, 1, 3)

    qh = heads(q)
    kh = heads(k) * (dh ** -0.5)
    vh = heads(v)
    gt = gates.astype(jnp.float32).reshape(B, T, 4, H).transpose(2, 0, 3, 1)
    h_fwd = _mlstm_scan(qh, kh, vh, gt[0], jax.nn.log_sigmoid(gt[1]))

    def flip(t):
        return jnp.flip(t, axis=2)

    h_bwd = flip(_mlstm_scan(flip(qh), flip(kh), flip(vh), flip(gt[2]), flip(jax.nn.log_sigmoid(gt[3]))))
    h = h_fwd + h_bwd
    mu = jnp.mean(h, axis=-1, keepdims=True)
    var = jnp.mean(jnp.square(h - mu), axis=-1, keepdims=True)
    h = (h - mu) * lax.rsqrt(var + EPS)
    h = h.transpose(0, 2, 1, 3).reshape(B, T, H * dh) * norm_w.astype(jnp.float32)
    return (jax.nn.sigmoid(o.astype(jnp.float32)) * h).astype(q.dtype)


def _neigh_attn(q, k, v, rpb):
    B, T, H, dh = q.shape
    rows = T // GRID_W
    kh = min(NA_KH, rows)
    qg = q.reshape(B, rows, GRID_W, H, dh) * (dh ** -0.5)
    kg = k.reshape(B, rows, GRID_W, H, dh)
    vg = v.reshape(B, rows, GRID_W, H, dh)
    cols = jnp.arange(GRID_W)
    cs = jnp.clip(cols - NA_KW // 2, 0, GRID_W - NA_KW)
    col_in = (cols[None, :] >= cs[:, None]) & (cols[None, :] < cs[:, None] + NA_KW)
    dc_idx = jnp.clip(cols[None, :] - cols[:, None], -(NA_KW - 1), NA_KW - 1) + NA_KW - 1

    def row_block(r):
        rs = jnp.clip(r - kh // 2, 0, rows - kh)
        kb = lax.dynamic_slice_in_dim(kg, rs, kh, axis=1).reshape(B, kh * GRID_W, H, dh)
        vb = lax.dynamic_slice_in_dim(vg, rs, kh, axis=1).reshape(B, kh * GRID_W, H, dh)
        qr = lax.dynamic_index_in_dim(qg, r, axis=1, keepdims=False)
        dr_idx = rs + jnp.arange(kh) - r + NA_KH - 1
        bias = rpb[:, dr_idx][:, :, dc_idx].astype(jnp.float32)
        bias = jnp.where(col_in[None, None], bias, NEG_INF)
        bias = bias.transpose(0, 2, 1, 3).reshape(H, GRID_W, kh * GRID_W)
        s = jnp.einsum('bqhd,bkhd->bhqk', qr, kb).astype(jnp.float32) + bias[None]
        p = jax.nn.softmax(s, axis=-1)
        return jnp.einsum('bhqk,bkhd->bqhd', p.astype(vb.dtype), vb)

    out = lax.map(row_block, jnp.arange(rows))
    return jnp.transpose(out, (1, 0, 2, 3, 4)).reshape(B, T, H * dh)


def _lin_combine(e1, e2):
    a1, b1 = e1
    a2, b2 = e2
    return (a1 * a2, a2 * b1 + b2)


def _rglru_branch(x, y, conv_w, conv_b, wa, ba, wx, bx, lam):
    B, T, W = x.shape
    xf = x.astype(jnp.float32)
    pad = CONV_W // 2
    xp = jnp.pad(xf, ((0, 0), (pad, CONV_W - 1 - pad), (0, 0)))
    xc = conv_b.astype(jnp.float32)
    for j in range(CONV_W):
        xc = xc + xp[:, j:j + T] * conv_w[j].astype(jnp.float32)
    xb = xc.reshape(B, T, LRU_BLOCKS, LRU_BLOCK)

    def direction(d, rev):
        r = jax.nn.sigmoid(jnp.einsum('btnk,nkj->btnj', xb, wa[d].astype(jnp.float32)).reshape(B, T, W) + ba[d])
        i = jax.nn.sigmoid(jnp.einsum('btnk,nkj->btnj', xb, wx[d].astype(jnp.float32)).reshape(B, T, W) + bx[d])
        log_a = -LRU_C * r * jax.nn.softplus(-lam[d].astype(jnp.float32))
        a = jnp.exp(log_a)
        bterm = jnp.sqrt(-jnp.expm1(2.0 * log_a)) * (i * xc)
        if rev:
            a = jnp.flip(a, axis=1)
            bterm = jnp.flip(bterm, axis=1)
        _, h = lax.associative_scan(_lin_combine, (a, bterm), axis=1)
        if rev:
            h = jnp.flip(h, axis=1)
        return h

    h = direction(0, False) + direction(1, True)
    return (h * jax.nn.gelu(y.astype(jnp.float32))).astype(x.dtype)


def _token_mixer(xm, w_in, b_in, mlstm_norm_w, na_rpb, conv_w, conv_b, lru_wa, lru_ba, lru_wx, lru_bx, lru_L, w_br_a, w_br_b, w_br_c, w_out):
    B, T, _ = xm.shape
    proj = jnp.einsum('btd,dn->btn', xm, w_in) + b_in
    aq, ak, av, ao, agates, bq, bk, bv, cx, cy, mg = _split_in(proj)
    h_a = _mlstm_bidir(aq, ak, av, ao, agates, mlstm_norm_w)
    h_b = _neigh_attn(bq.reshape(B, T, NA_HEADS, NA_HEAD_DIM), bk.reshape(B, T, NA_HEADS, NA_HEAD_DIM), bv.reshape(B, T, NA_HEADS, NA_HEAD_DIM), na_rpb)
    h_c = _rglru_branch(cx, cy, conv_w, conv_b, lru_wa, lru_ba, lru_wx, lru_bx, lru_L)
    g = jax.nn.sigmoid(mg.astype(jnp.float32)).reshape(B, T, N_BRANCH, D_MODEL).astype(xm.dtype)
    merged = g[:, :, 0] * (h_a @ w_br_a) + g[:, :, 1] * (h_b @ w_br_b) + g[:, :, 2] * (h_c @ w_br_c)
    return merged @ w_out


def _expert_choice(xm, w_router, b_router, w_g, w_u, w_d):
    B, T, D = xm.shape
    N = B * T
    xf = xm.reshape(N, D)
    cap = EC_FACTOR * N // N_EXPERTS
    logits = (xf @ w_router).astype(jnp.float32) + b_router.astype(jnp.float32)
    aff = jax.nn.softmax(logits, axis=-1)
    gate, idx = lax.top_k(aff.T, cap)
    xe = jnp.take(xf, idx, axis=0)
    hid = jax.nn.silu(jnp.einsum('ecd,edf->ecf', xe, w_g)) * jnp.einsum('ecd,edf->ecf', xe, w_u)
    ye = jnp.einsum('ecf,efd->ecd', hid, w_d) * gate[..., None].astype(xm.dtype)
    out = jnp.zeros_like(xf).at[idx.reshape(-1)].add(ye.reshape(-1, D))
    return out.reshape(B, T, D)


def _trunk(x, c, norm1_w, norm2_w, w_mod, b_mod, w_in, b_in, mlstm_norm_w, na_rpb, conv_w, conv_b, lru_wa, lru_ba, lru_wx, lru_bx, lru_L, w_br_a, w_br_b, w_br_c, w_out, w_router, b_router, w_gate_e, w_up_e, w_down_e, final_norm_w):
    c_act = jax.nn.silu(c)
    for l in range(DEPTH):
        mod = (c_act @ w_mod[l] + b_mod[l])[:, None, :]
        sh1, sc1, g1, sh2, sc2, g2 = jnp.split(mod, N_MOD, axis=-1)
        xm = _rmsnorm(x, norm1_w[l]) * (1.0 + sc1) + sh1
        x = x + g1 * _token_mixer(xm, w_in[l], b_in[l], mlstm_norm_w[l], na_rpb[l], conv_w[l], conv_b[l], lru_wa[l], lru_ba[l], lru_wx[l], lru_bx[l], lru_L[l], w_br_a[l], w_br_b[l], w_br_c[l], w_out[l])
        xm = _rmsnorm(x, norm2_w[l]) * (1.0 + sc2) + sh2
        x = x + g2 * _expert_choice(xm, w_router[l], b_router[l], w_gate_e[l], w_up_e[l], w_down_e[l])
    return _rmsnorm(x, final_norm_w)


def setup_inputs(seed: int = 0) -> dict:
    key = jax.random.key(seed)
    ks = jax.random.split(key, 32)
    f32 = jnp.float32

    def nrm(k, shape, scale):
        return jax.random.normal(k, shape, f32) * scale

    x_prompt = nrm(ks[0], (BATCH, SEQ, D_MODEL), 1.0)
    x_sample = nrm(ks[1], (DEC_BATCH, DEC_SEQ, D_MODEL), 1.0)
    c_prompt = nrm(ks[2], (BATCH, D_MODEL), 1.0)
    c_sample = nrm(ks[3], (DEC_BATCH, D_MODEL), 1.0)
    norm1_w = 1.0 + nrm(ks[4], (DEPTH, D_MODEL), 0.02)
    norm2_w = 1.0 + nrm(ks[5], (DEPTH, D_MODEL), 0.02)
    w_mod = nrm(ks[6], (DEPTH, D_MODEL, N_MOD * D_MODEL), 0.5 * D_MODEL ** -0.5)
    b_mod = nrm(ks[7], (DEPTH, N_MOD * D_MODEL), 0.02)
    w_in = nrm(ks[8], (DEPTH, D_MODEL, N_IN), D_MODEL ** -0.5)
    b_in = nrm(ks[9], (DEPTH, N_IN), 0.02)
    off = 4 * MLSTM_WIDTH
    f_bias = jnp.linspace(3.0, 6.0, MLSTM_HEADS, dtype=f32)
    b_in = b_in.at[:, off + MLSTM_HEADS:off + 2 * MLSTM_HEADS].add(f_bias)
    b_in = b_in.at[:, off + 3 * MLSTM_HEADS:off + 4 * MLSTM_HEADS].add(f_bias)
    mlstm_norm_w = 1.0 + nrm(ks[10], (DEPTH, MLSTM_WIDTH), 0.02)
    na_rpb = nrm(ks[11], (DEPTH, NA_HEADS, 2 * NA_KH - 1, 2 * NA_KW - 1), 0.1)
    conv_w = nrm(ks[12], (DEPTH, CONV_W, LRU_WIDTH), CONV_W ** -0.5)
    conv_b = nrm(ks[13], (DEPTH, LRU_WIDTH), 0.02)
    lru_wa = nrm(ks[14], (DEPTH, 2, LRU_BLOCKS, LRU_BLOCK, LRU_BLOCK), LRU_BLOCK ** -0.5)
    lru_ba = nrm(ks[15], (DEPTH, 2, LRU_WIDTH), 0.02)
    lru_wx = nrm(ks[16], (DEPTH, 2, LRU_BLOCKS, LRU_BLOCK, LRU_BLOCK), LRU_BLOCK ** -0.5)
    lru_bx = nrm(ks[17], (DEPTH, 2, LRU_WIDTH), 0.02)
    u = jax.random.uniform(ks[18], (DEPTH, 2, LRU_WIDTH), f32, minval=0.9, maxval=0.999)
    a0 = u ** (1.0 / LRU_C)
    lru_L = jnp.log(a0) - jnp.log1p(-a0)
    w_br_a = nrm(ks[19], (DEPTH, MLSTM_WIDTH, D_MODEL), MLSTM_WIDTH ** -0.5)
    w_br_b = nrm(ks[20], (DEPTH, NA_WIDTH, D_MODEL), NA_WIDTH ** -0.5)
    w_br_c = nrm(ks[21], (DEPTH, LRU_WIDTH, D_MODEL), LRU_WIDTH ** -0.5)
    w_out = nrm(ks[22], (DEPTH, D_MODEL, D_MODEL), D_MODEL ** -0.5)
    w_router = nrm(ks[23], (DEPTH, D_MODEL, N_EXPERTS), D_MODEL ** -0.5)
    b_router = nrm(ks[24], (DEPTH, N_EXPERTS), 0.01)
    w_gate_e = nrm(ks[25], (DEPTH, N_EXPERTS, D_MODEL, D_EXPERT), D_MODEL ** -0.5)
    w_up_e = nrm(ks[26], (DEPTH, N_EXPERTS, D_MODEL, D_EXPERT), D_MODEL ** -0.5)
    w_down_e = nrm(ks[27], (DEPTH, N_EXPERTS, D_EXPERT, D_MODEL), D_EXPERT ** -0.5)
    final_norm_w = 1.0 + nrm(ks[28], (D_MODEL,), 0.02)
    return {'x_prompt': x_prompt, 'x_sample': x_sample, 'c_prompt': c_prompt, 'c_sample': c_sample,
            'norm1_w': norm1_w, 'norm2_w': norm2_w, 'w_mod': w_mod, 'b_mod': b_mod, 'w_in': w_in, 'b_in': b_in,
            'mlstm_norm_w': mlstm_norm_w, 'na_rpb': na_rpb, 'conv_w': conv_w, 'conv_b': conv_b,
            'lru_wa': lru_wa, 'lru_ba': lru_ba, 'lru_wx': lru_wx, 'lru_bx': lru_bx, 'lru_L': lru_L,
            'w_br_a': w_br_a, 'w_br_b': w_br_b, 'w_br_c': w_br_c, 'w_out': w_out,
            'w_router': w_router, 'b_router': b_router, 'w_gate_e': w_gate_e, 'w_up_e': w_up_e, 'w_down_e': w_down_e,
            'final_norm_w': final_norm_w}


def reference(x_prompt, x_sample, c_prompt, c_sample, norm1_w, norm2_w, w_mod, b_mod, w_in, b_in, mlstm_norm_w, na_rpb, conv_w, conv_b, lru_wa, lru_ba, lru_wx, lru_bx, lru_L, w_br_a, w_br_b, w_br_c, w_out, w_router, b_router, w_gate_e, w_up_e, w_down_e, final_norm_w):
    y_prompt = _trunk(x_prompt, c_prompt, norm1_w, norm2_w, w_mod, b_mod, w_in, b_in, mlstm_norm_w, na_rpb, conv_w, conv_b, lru_wa, lru_ba, lru_wx, lru_bx, lru_L, w_br_a, w_br_b, w_br_c, w_out, w_router, b_router, w_gate_e, w_up_e, w_down_e, final_norm_w)
    y_sample = _trunk(x_sample, c_sample, norm1_w, norm2_w, w_mod, b_mod, w_in, b_in, mlstm_norm_w, na_rpb, conv_w, conv_b, lru_wa, lru_ba, lru_wx, lru_bx, lru_L, w_br_a, w_br_b, w_br_c, w_out, w_router, b_router, w_gate_e, w_up_e, w_down_e, final_norm_w)
    return (y_prompt, y_sample)
```

```python
import numpy as np
from contextlib import ExitStack
import concourse.bass as bass
import concourse.mybir as mybir
from concourse.bass_utils import run_bass_kernel_spmd

F32 = mybir.dt.float32
BF16 = mybir.dt.bfloat16
I32 = mybir.dt.int32
ALU = mybir.AluOpType
AF = mybir.ActivationFunctionType
AX = mybir.AxisListType

D = 1024
KC = 8
NSEG = 4
N_IN = 12304
OFF = dict(aq=0, ak=1024, av=2048, ao=3072, ag=4096, bq=4112, bk=5136, bv=6160, cx=7184, cy=8208, mg=9232)
NEXP = 16
DEXP = 2048
EPS = 1e-6
NEGM = -30000.0


class Tok:
    __slots__ = ("w", "r", "name")

    def __init__(self, name=""):
        self.w = None
        self.r = []
        self.name = name


class T:
    def __init__(self, t, tok):
        self.t = t
        self.tok = tok

    def __getitem__(self, k):
        return self.t[k]


class Ctx:
    ENG = ("pe", "act", "dve", "pool", "sp")

    def __init__(self, nc, n_dma_sems=32):
        self.nc = nc
        self.es = ExitStack()
        self.scopes = []
        self.eng = {"pe": nc.tensor, "act": nc.scalar, "dve": nc.vector, "pool": nc.gpsimd, "sp": nc.sync}
        self.sem = {}
        self.cnt = {}
        for e in self.ENG:
            self.sem[e] = self.es.enter_context(nc.semaphore("s_" + e))
            self.cnt[e] = 0
        self.dsem = []
        self.dcnt = []
        for i in range(n_dma_sems):
            self.dsem.append(self.es.enter_context(nc.semaphore("d%d" % i)))
            self.dcnt.append(0)
        self.dnext = 0
        self.ccsem = self.es.enter_context(nc.semaphore("ccsem"))
        self.cccnt = 0
        self.known = {e: {} for e in self.ENG}
        self.ninst = 0

    def _stack(self):
        return self.scopes[-1] if self.scopes else self.es

    def push(self):
        self.scopes.append(ExitStack())

    def pop(self):
        self.barrier()
        self.scopes.pop().close()

    def sbuf(self, name, shape, dtype):
        self.uid = getattr(self, "uid", 0) + 1
        t = self._stack().enter_context(self.nc.sbuf_tensor("sb%d_%s" % (self.uid, name), list(shape), dtype))
        return T(t, Tok(name))

    def psum(self, name, shape, dtype):
        self.uid = getattr(self, "uid", 0) + 1
        t = self._stack().enter_context(self.nc.psum_tensor("ps%d_%s" % (self.uid, name), list(shape), dtype))
        return T(t, Tok(name))

    def dram(self, name, shape, dtype, kind="Internal"):
        t = self.nc.dram_tensor(name, list(shape), dtype, kind=kind)
        return T(t.ap(), Tok(name))

    def _semobj(self, key):
        if key == "cc":
            return self.ccsem
        if isinstance(key, str):
            return self.sem[key]
        return self.dsem[key]

    def _wait(self, e, key, val):
        k = self.known[e]
        if k.get(key, 0) >= val:
            return
        self.eng[e].wait_ge(self._semobj(key), val)
        k[key] = val

    def _deps(self, e, reads, writes):
        need = {}

        def add(d, same_ok):
            key, val, de = d
            if de == e and same_ok:
                return
            if need.get(key, 0) < val:
                need[key] = val
        for t in reads:
            if t.w is not None:
                add(t.w, False)
        for t in writes:
            if t.w is not None:
                add(t.w, True)
            for d in t.r:
                add(d, True)
        for key, val in need.items():
            self._wait(e, key, val)

    def _mark(self, reads, writes, d):
        for t in writes:
            t.w = d
            t.r = []
        for t in reads:
            if t in writes:
                continue
            t.r = [x for x in t.r if x[0] != d[0]]
            t.r.append(d)

    @staticmethod
    def _toks(lst):
        out = []
        for x in lst:
            if x is None:
                continue
            out.append(x.tok if isinstance(x, T) else x)
        return out

    def op(self, e, fn, reads=(), writes=(), inc=True):
        reads = self._toks(reads)
        writes = self._toks(writes)
        self._deps(e, reads, writes)
        inst = fn()
        self.ninst += 1
        if inc:
            self.cnt[e] += 1
            inst.then_inc(self.sem[e], 1)
            val = self.cnt[e]
        else:
            val = self.cnt[e] + 1
        self._mark(reads, writes, (e, val, e))
        return inst

    def dma(self, q, out, in_, reads=(), writes=(), **kw):
        reads = self._toks(reads)
        writes = self._toks(writes)
        self._deps(q, reads, writes)
        i = self.dnext
        self.dnext = (self.dnext + 1) % len(self.dsem)
        if self.dcnt[i] > 0:
            self._wait(q, i, self.dcnt[i])
        inst = self.eng[q].dma_start(out=out, in_=in_, **kw)
        self.ninst += 1
        self.dcnt[i] += 16
        inst.then_inc(self.dsem[i], 16)
        self._mark(reads, writes, (i, self.dcnt[i], "dma"))
        return inst

    def allgather(self, in_t, out_t, groups):
        reads = [in_t.tok]
        writes = [out_t.tok]
        self._deps("pool", reads, writes)
        inst = self.nc.gpsimd.collective_compute("AllGather", op=ALU.bypass, replica_groups=groups,
                                                 ins=[in_t.t], outs=[out_t.t])
        self.cccnt += 1
        inst.then_inc(self.ccsem, 1)
        self._mark(reads, writes, ("cc", self.cccnt, "cc"))

    def barrier(self):
        for e in self.ENG:
            for x in self.ENG:
                if x != e and self.cnt[x] > 0:
                    self._wait(e, x, self.cnt[x])
            for i in range(len(self.dsem)):
                if self.dcnt[i] > 0:
                    self._wait(e, i, self.dcnt[i])
            if self.cccnt:
                self._wait(e, "cc", self.cccnt)

    def close(self):
        self.barrier()
        while self.scopes:
            self.scopes.pop().close()
        self.es.close()


class Pool:
    def __init__(self, c, name, shape, dtype, n, psum=False):
        self.tiles = [(c.psum if psum else c.sbuf)("%s%d" % (name, i), shape, dtype) for i in range(n)]
        self.i = 0

    def next(self):
        t = self.tiles[self.i]
        self.i = (self.i + 1) % len(self.tiles)
        return t


def build(Tn, cap, stop_after=None, dbg=(), DEXP=DEXP):
    with_moe = stop_after is None or stop_after in ("moe",)
    nc = bass.Bass("TRN2", target_bir_lowering=False)
    c = Ctx(nc)
    TSEG = Tn // NSEG
    NTT = Tn // 128
    NBLK = Tn // 512
    ROWS = Tn // 64
    CPS = TSEG // 128

    def din(name, shape, dt=F32):
        return T(nc.dram_tensor(name, list(shape), dt, kind="ExternalInput").ap(), Tok(name))

    x_in = din("x", [Tn, D])
    c4T = din("c4T", [128, KC, NSEG])
    keep_in = din("keep", [128, 1])
    rv_in = din("rv", [128, ROWS * 8])
    norm1_w = din("norm1_w", [2, D]); norm2_w = din("norm2_w", [2, D])
    GROUPS = [[0, 1, 2, 3], [4, 5, 6, 7]]
    b_mod = din("b_mod", [2, 6 * D])
    b_in = din("b_in", [2, N_IN])
    mlstm_norm_w = din("mlstm_norm_w", [2, D])
    btab = din("btab", [2, 128, 16 * 16 * 64])
    conv_w = din("conv_w", [2, 4, D]); conv_b = din("conv_b", [2, D])
    lru_bd = din("lru_bd", [2, 4, 8, 128, 128])
    lru_ba = din("lru_ba", [2, 2, D]); lru_bx = din("lru_bx", [2, 2, D]); lru_L = din("lru_L", [2, 2, D])
    w_router = din("w_router", [2, D, NEXP]); b_router = din("b_router", [2, NEXP])
    w_in = din("w_in", [2, D, N_IN])
    w_mod = din("w_mod", [2, D, 6 * D])
    w_br = [din("w_br_a", [2, D, D]), din("w_br_b", [2, D, D]), din("w_br_c", [2, D, D])]
    w_out = din("w_out", [2, D, D])
    if with_moe:
        EG = din("w_gate_e", [2, NEXP, D, DEXP]); EU = din("w_up_e", [2, NEXP, D, DEXP]); ED = din("w_down_e", [2, NEXP, DEXP, D])

        def expert_w(t_, l, e, nrows):
            return T(t_.t[l, e], t_.tok)
    final_norm_w = din("final_norm_w", [1, D])
    y_out = T(nc.dram_tensor("y", [Tn, D], F32, kind="ExternalOutput").ap(), Tok("y"))

    X = [c.dram("Xs0", [Tn, D], F32), c.dram("Xs1", [Tn, D], F32)]
    MOD = c.dram("MOD", [2, NSEG, 6 * D], F32)
    XMT = c.dram("XMT", [KC, 128, Tn], BF16)
    QKT = c.dram("QKT", [16, 128, Tn], BF16)
    KVO = c.dram("KVO", [Tn, 3 * D], BF16)
    HF = c.dram("HF", [Tn, D], F32)
    HT = [c.dram("HAT", [KC, 128, Tn], BF16), c.dram("HBT", [KC, 128, Tn], BF16), c.dram("HCT", [KC, 128, Tn], BF16)]
    NV = c.dram("NV", [Tn, D], BF16)
    CXY = c.dram("CXY", [16, 128, Tn], F32)
    AFF = c.dram("AFF", [Tn, NEXP], F32)
    AFFALL = c.dram("AFFALL", [4 * Tn, NEXP], F32)

    if with_moe:
        NFQ = DEXP // 512
        EGb = c.dram("EGb", [2 * NEXP * NFQ, 128, 4096], BF16)
        EUb = c.dram("EUb", [2 * NEXP * NFQ, 128, 4096], BF16)
        EDb = c.dram("EDb", [2 * NEXP * NFQ, 128, 4096], BF16)
    dbg_out = {}

    def dbg_dump(name, src_t, shape, dt=F32):
        if name in dbg:
            o = T(nc.dram_tensor("dbg_" + name, list(shape), dt, kind="ExternalOutput").ap(), Tok("dbg_" + name))
            c.dma("sp", o.t, src_t.t, reads=[src_t], writes=[o])
            dbg_out[name] = o

    ident_f = c.sbuf("ident_f", [128, 128], F32)
    ident_b = c.sbuf("ident_b", [128, 128], BF16)
    triu_f = c.sbuf("triu_f", [128, 128], F32)
    tril_f = c.sbuf("tril_f", [128, 128], F32)
    mask_f = c.sbuf("mask_f", [128, 128], BF16)
    mask_b = c.sbuf("mask_b", [128, 128], BF16)
    ones_f = c.sbuf("ones_f", [128, 128], F32)
    ones_b = c.sbuf("ones_b", [128, 128], BF16)
    keep = c.sbuf("keep", [128, 1], F32)
    nkeep = c.sbuf("nkeep", [128, 1], F32)

    def pool_op(fn, reads=(), writes=()):
        return c.op("pool", fn, reads, writes)

    def dve(fn, reads=(), writes=()):
        return c.op("dve", fn, reads, writes)

    def act(fn, reads=(), writes=()):
        return c.op("act", fn, reads, writes)

    pool_op(lambda: nc.gpsimd.memset(ones_f[:], 1.0), writes=[ones_f])
    pool_op(lambda: nc.gpsimd.memset(ones_b[:], 1.0), writes=[ones_b])
    pool_op(lambda: nc.gpsimd.affine_select(out=ident_f[:], in_=ones_f[:], pattern=[[-1, 128]], compare_op=ALU.is_equal,
                                            fill=0.0, base=0, channel_multiplier=1), reads=[ones_f], writes=[ident_f])
    pool_op(lambda: nc.gpsimd.affine_select(out=triu_f[:], in_=ones_f[:], pattern=[[1, 128]], compare_op=ALU.is_ge,
                                            fill=0.0, base=0, channel_multiplier=-1), reads=[ones_f], writes=[triu_f])
    pool_op(lambda: nc.gpsimd.affine_select(out=tril_f[:], in_=ones_f[:], pattern=[[-1, 128]], compare_op=ALU.is_ge,
                                            fill=0.0, base=0, channel_multiplier=1), reads=[ones_f], writes=[tril_f])
    dve(lambda: nc.vector.tensor_copy(out=ident_b[:], in_=ident_f[:]), [ident_f], [ident_b])
    dve(lambda: nc.vector.tensor_copy(out=mask_f[:], in_=triu_f[:]), [triu_f], [mask_f])
    dve(lambda: nc.vector.tensor_copy(out=mask_b[:], in_=tril_f[:]), [tril_f], [mask_b])
    c.dma("sp", keep[:], keep_in.t, writes=[keep])
    dve(lambda: nc.vector.tensor_scalar(out=nkeep[:], in0=keep[:], scalar1=-1.0, scalar2=1.0, op0=ALU.mult, op1=ALU.add),
        [keep], [nkeep])

    PSF = Pool(c, "psf", [128, 512], F32, 5, psum=True)
    PSL = Pool(c, "psl", [128, 512], F32, 1, psum=True)

    def mm_group(ps, out_ap, pairs, reads):
        n = len(pairs)
        for i, (l, r) in enumerate(pairs):
            c.op("pe", (lambda l=l, r=r, i=i: nc.tensor.matmul(out_ap, lhsT=l, rhs=r, start=(i == 0), stop=(i == n - 1))),
                 reads=reads, writes=[ps], inc=(i == n - 1))

    def seg_of_tt(tt):
        return (tt * 128) // TSEG

    def precast_units(stg, stb):
        it = 0
        for l in range(2):
            for e in range(NEXP):
                for fq in range(NFQ):
                    idx = (l * NEXP + e) * NFQ + fq
                    for which in range(3):
                        sf = stg.next(); sb_ = stb.next()
                        if which == 0:
                            srcap = EG.t[l, e][:, fq * 512:(fq + 1) * 512].rearrange("(k p) n -> p k n", p=128); srct = EG; dstt = EGb
                        elif which == 1:
                            srcap = EU.t[l, e][:, fq * 512:(fq + 1) * 512].rearrange("(k p) n -> p k n", p=128); srct = EU; dstt = EUb
                        else:
                            srcap = ED.t[l, e][fq * 512:(fq + 1) * 512, :].rearrange("(k p) n -> p k n", p=128); srct = ED; dstt = EDb
                        if which < 2:
                            c.dma("sp", sf[:].rearrange("p (k n) -> p k n", k=KC), srcap, reads=[srct], writes=[sf])
                        else:
                            c.dma("sp", sf[:].rearrange("p (k n) -> p k n", k=4), srcap, reads=[srct], writes=[sf])
                        if it % 2 == 0:
                            act(lambda: nc.scalar.copy(out=sb_[:], in_=sf[:]), [sf], [sb_])
                        else:
                            dve(lambda: nc.vector.tensor_copy(out=sb_[:], in_=sf[:]), [sf], [sb_])
                        it += 1
                        c.dma("act", dstt.t[idx], sb_[:], reads=[sb_], writes=[dstt])
                        yield

    PRE = {"gen": None}

    c.push()
    cT = c.sbuf("cT", [128, KC, NSEG], F32)
    cTb = c.sbuf("cTb", [128, KC, NSEG], BF16)
    c.dma("sp", cT[:], c4T.t, writes=[cT])
    act(lambda: nc.scalar.activation(out=cTb[:], in_=cT[:], func=AF.Silu), [cT], [cTb])
    wmp = Pool(c, "wmod", [128, KC, 512], BF16, 2)
    modsb = c.sbuf("modsb", [NSEG, 6 * D], F32)
    bmodb = c.sbuf("bmodb", [NSEG, 6 * D], F32)
    for l in range(2):
        c.dma("sp", bmodb[:], b_mod.t[l:l + 1, :].partition_broadcast(NSEG) if False else b_mod.t[l:l + 1, :].to_broadcast([NSEG, 6 * D]),
              reads=[b_mod], writes=[bmodb])
        for nb in range(12):
            wt = wmp.next()
            c.dma("pool", wt[:], w_mod.t[l, :, nb * 512:(nb + 1) * 512].rearrange("(k p) n -> p k n", p=128),
                  reads=[w_mod], writes=[wt])
            ps = PSF.next()
            mm_group(ps, ps[0:NSEG, :], [(cTb[:, kc, :], wt[:, kc, :]) for kc in range(KC)], [cTb, wt])
            dve(lambda: nc.vector.tensor_tensor(out=modsb[:, nb * 512:(nb + 1) * 512], in0=ps[0:NSEG, :],
                                                in1=bmodb[:, nb * 512:(nb + 1) * 512], op=ALU.add), [ps, bmodb], [modsb])
        c.dma("act", MOD.t[l], modsb[:], reads=[modsb], writes=[MOD])
    c.pop()
    modT = c.sbuf("modT", [128, 2 * 6 * KC * NSEG], F32)

    def modT_ap(l, k, kc, seg):
        o = ((l * 6 + k) * NSEG + seg) * KC + kc
        return modT[:, o:o + 1]
    with nc.allow_non_contiguous_dma(reason="tiny modulation transpose load"):
        for l in range(2):
            for k in range(6):
                for seg in range(NSEG):
                    o = ((l * 6 + k) * NSEG + seg) * KC
                    c.dma("sp", modT[:, o:o + KC], MOD.t[l, seg, k * D:(k + 1) * D].rearrange("(k p) -> p k", p=128),
                          reads=[MOD], writes=[modT])
    dbg_dump("MOD", MOD, [2, NSEG, 6 * D])

    def phase_norm(l, which, src):
        c.push()
        PSB = Pool(c, "psb", [128, 1024], BF16, 2, psum=True)
        nw = norm1_w if which == 0 else norm2_w
        ksh, ksc = (0, 1) if which == 0 else (3, 4)
        nwT = c.sbuf("nwT", [128, KC], F32)
        with nc.allow_non_contiguous_dma(reason="tiny"):
            c.dma("sp", nwT[:], nw.t[l, :].rearrange("(k p) -> p k", p=128), reads=[nw], writes=[nwT])
        Asc = c.sbuf("Asc", [128, KC * NSEG], F32)
        for seg in range(NSEG):
            o = ((l * 6 + ksc) * NSEG + seg) * KC
            dve(lambda: nc.vector.scalar_tensor_tensor(out=Asc[:, seg * KC:(seg + 1) * KC], in0=modT[:, o:o + KC], scalar=1.0,
                                                       in1=nwT[:], op0=ALU.add, op1=ALU.mult), [modT, nwT], [Asc])
        xp = Pool(c, "xa", [128, D], F32, 3)
        junk = c.sbuf("junk", [128, D], BF16)
        xnp = Pool(c, "xn", [128, D], BF16, 2)
        ssp = Pool(c, "ss", [128, 2], F32, 4)
        blkp = Pool(c, "xmblk", [128, KC, 512], BF16, 2)
        blk = None
        for tt in range(NTT):
            seg = seg_of_tt(tt)
            if tt % 4 == 0:
                blk = blkp.next()
            xt = xp.next()
            c.dma("sp", xt[:], src.t[tt * 128:(tt + 1) * 128, :], reads=[src], writes=[xt])
            ss = ssp.next()
            act(lambda: nc.scalar.activation(out=junk[:], in_=xt[:], func=AF.Square, accum_out=ss[:, 0:1]), [xt], [junk, ss])
            act(lambda: nc.scalar.activation(out=ss[:, 1:2], in_=ss[:, 0:1], func=AF.Sqrt, bias=EPS, scale=1.0 / D), [ss], [ss])
            dve(lambda: nc.vector.reciprocal(out=ss[:, 1:2], in_=ss[:, 1:2]), [ss], [ss])
            xn = xnp.next()
            dve(lambda: nc.vector.tensor_scalar(out=xn[:], in0=xt[:], scalar1=ss[:, 1:2], scalar2=None, op0=ALU.mult), [xt, ss], [xn])
            pb = PSB.next()
            for kc in range(KC):
                c.op("pe", lambda kc=kc: nc.tensor.transpose(pb[:, kc * 128:(kc + 1) * 128], xn[:, kc * 128:(kc + 1) * 128], ident_b[:]),
                     reads=[xn, ident_b], writes=[pb], inc=(kc == KC - 1))
            for kc in range(KC):
                a_ap = Asc[:, seg * KC + kc:seg * KC + kc + 1]
                b_ap = modT_ap(l, ksh, kc, seg)
                o_ap = blk[:, kc, (tt % 4) * 128:(tt % 4 + 1) * 128]
                i_ap = pb[:, kc * 128:(kc + 1) * 128]
                if kc % 2 == 0:
                    dve(lambda: nc.vector.tensor_scalar(out=o_ap, in0=i_ap, scalar1=a_ap, scalar2=b_ap, op0=ALU.mult, op1=ALU.add),
                        [pb, Asc, modT], [blk])
                else:
                    act(lambda: nc.scalar.activation(out=o_ap, in_=i_ap, func=AF.Identity, bias=b_ap, scale=a_ap), [pb, Asc, modT], [blk])
            if tt % 4 == 3:
                b0 = (tt // 4) * 512
                c.dma("act", XMT.t[:, :, b0:b0 + 512].rearrange("k p t -> p k t"), blk[:], reads=[blk], writes=[XMT])
        c.pop()

    def load_w(pool_t, l, col0, ncols):
        c.dma("pool", pool_t[:, :, 0:ncols], w_in.t[l, :, col0:col0 + ncols].rearrange("(k p) n -> p k n", p=128),
              reads=[w_in], writes=[pool_t])

    def load_bias_fm(t, l, col0, nft):
        with nc.allow_non_contiguous_dma(reason="tiny bias"):
            c.dma("sp", t[:, 0:nft], b_in.t[l, col0:col0 + nft * 128].rearrange("(k p) -> p k", p=128), reads=[b_in], writes=[t])

    def load_bias_row(t, l, col0, ncols):
        c.dma("sp", t[:, 0:ncols], b_in.t[l:l + 1, col0:col0 + ncols].to_broadcast([128, ncols]), reads=[b_in], writes=[t])

    def load_xmblk(t, blk):
        c.dma("sp", t[:], XMT.t[:, :, blk * 512:(blk + 1) * 512].rearrange("k p t -> p k t"), reads=[XMT], writes=[t])

    def phase_mlstm_proj(l, GA):
        c.push()
        Wqk = c.sbuf("Wqk", [128, KC, 2048], BF16)
        Wkvo = c.sbuf("Wkvo", [128, KC, 3072], BF16)
        Wg = c.sbuf("Wg", [128, KC, 16], BF16)
        load_w(Wqk, l, OFF["aq"], 2048)
        load_w(Wkvo, l, OFF["ak"], 3072)
        load_w(Wg, l, OFF["ag"], 16)
        bqk = c.sbuf("bqk", [128, 16], F32)
        load_bias_fm(bqk, l, OFF["aq"], 16)
        brow = c.sbuf("brow", [128, 3072 + 16], F32)
        load_bias_row(brow, l, OFF["ak"], 3072 + 16)
        xbp = Pool(c, "xb", [128, KC, 512], BF16, 2)
        stq = Pool(c, "stq", [128, 16, 512], BF16, 2)
        stk = Pool(c, "stk", [128, 3072], BF16, 2)
        tmpo = Pool(c, "tmpo", [128, 512], F32, 2)
        for blk in range(NBLK):
            xb = xbp.next()
            load_xmblk(xb, blk)
            sq = stq.next()
            for ft in range(16):
                ps = PSF.next()
                mm_group(ps, ps[:], [(Wqk[:, kc, ft * 128:(ft + 1) * 128], xb[:, kc, :]) for kc in range(KC)], [Wqk, xb])
                if ft % 2 == 0:
                    act(lambda: nc.scalar.activation(out=sq[:, ft, :], in_=ps[:], func=AF.Identity, bias=bqk[:, ft:ft + 1], scale=1.0),
                        [ps, bqk], [sq])
                else:
                    dve(lambda: nc.vector.tensor_scalar(out=sq[:, ft, :], in0=ps[:], scalar1=bqk[:, ft:ft + 1], scalar2=None, op0=ALU.add),
                        [ps, bqk], [sq])
            c.dma("act", QKT.t[:, :, blk * 512:(blk + 1) * 512].rearrange("k p t -> p k t"), sq[:], reads=[sq], writes=[QKT])
            for t4 in range(4):
                tt = blk * 4 + t4
                sk = stk.next()
                for nh in range(6):
                    ps = PSF.next()
                    mm_group(ps, ps[:], [(xb[:, kc, t4 * 128:(t4 + 1) * 128], Wkvo[:, kc, nh * 512:(nh + 1) * 512]) for kc in range(KC)], [Wkvo, xb])
                    if nh < 4:
                        dve(lambda: nc.vector.tensor_tensor(out=sk[:, nh * 512:(nh + 1) * 512], in0=ps[:], in1=brow[:, nh * 512:(nh + 1) * 512], op=ALU.add),
                            [ps, brow], [sk])
                    else:
                        tp = tmpo.next()
                        dve(lambda: nc.vector.tensor_tensor(out=tp[:], in0=ps[:], in1=brow[:, nh * 512:(nh + 1) * 512], op=ALU.add), [ps, brow], [tp])
                        act(lambda: nc.scalar.activation(out=sk[:, nh * 512:(nh + 1) * 512], in_=tp[:], func=AF.Sigmoid), [tp], [sk])
                c.dma("act", KVO.t[tt * 128:(tt + 1) * 128, :], sk[:], reads=[sk], writes=[KVO])
                ps = PSF.next()
                mm_group(ps, ps[:, 0:16], [(xb[:, kc, t4 * 128:(t4 + 1) * 128], Wg[:, kc, :]) for kc in range(KC)], [Wg, xb])
                dve(lambda: nc.vector.tensor_tensor(out=GA[:, tt, :], in0=ps[:, 0:16], in1=brow[:, 3072:3088], op=ALU.add), [ps, brow], [GA])
        c.pop()

    def phase_mlstm_scan(l, GA):
        c.push()
        PSB = Pool(c, "psb", [128, 1024], BF16, 2, psum=True)
        LF = c.sbuf("LF", [128, NTT, 8], F32)
        IG = c.sbuf("IG", [128, NTT, 8], F32)
        BC = c.sbuf("BC", [128, NTT, 8], F32)
        E1 = c.sbuf("E1", [128, NTT, 8], F32)
        for d in range(2):
            dve(lambda: nc.vector.tensor_copy(out=IG[:, :, d * 4:(d + 1) * 4], in_=GA[:, :, d * 8:d * 8 + 4]), [GA], [IG])
            act(lambda: nc.scalar.activation(out=LF[:, :, d * 4:(d + 1) * 4], in_=GA[:, :, d * 8 + 4:d * 8 + 8], func=AF.Exp, scale=-1.0), [GA], [LF])
        act(lambda: nc.scalar.activation(out=LF[:], in_=LF[:], func=AF.Ln, bias=1.0, scale=1.0), [LF], [LF])
        dve(lambda: nc.vector.tensor_scalar(out=LF[:], in0=LF[:], scalar1=-1.0, scalar2=None, op0=ALU.mult), [LF], [LF])
        CW = 512 // 8
        for d in range(2):
            tri = triu_f if d == 0 else tril_f
            for c0 in range(0, NTT, CW):
                c1 = min(NTT, c0 + CW)
                n = (c1 - c0) * 4
                ps = PSF.next()
                for cc in range(c0, c1):
                    c.op("pe", lambda cc=cc: nc.tensor.matmul(ps[:, (cc - c0) * 4:(cc - c0 + 1) * 4], lhsT=tri[:], rhs=LF[:, cc, d * 4:(d + 1) * 4], start=True, stop=True),
                         reads=[tri, LF], writes=[ps], inc=(cc == c1 - 1))
                dve(lambda: nc.vector.tensor_copy(out=BC[:, c0:c1, d * 4:(d + 1) * 4], in_=ps[:, 0:n].rearrange("p (c h) -> p c h", h=4)), [ps], [BC])
        dve(lambda: nc.vector.tensor_tensor(out=E1[:], in0=IG[:], in1=BC[:], op=ALU.subtract), [IG, BC], [E1])
        act(lambda: nc.scalar.activation(out=E1[:], in_=E1[:], func=AF.Exp), [E1], [E1])
        dve(lambda: nc.vector.tensor_scalar(out=E1[:], in0=E1[:], scalar1=1.0 / 16.0, scalar2=None, op0=ALU.mult), [E1], [E1])

        nwb = c.sbuf("nwb", [128, D], F32)
        c.dma("sp", nwb[:], mlstm_norm_w.t[l:l + 1, :].to_broadcast([128, D]), reads=[mlstm_norm_w], writes=[nwb])
        qtp = Pool(c, "qT", [128, 8, 128], BF16, 2)
        ktp = Pool(c, "kT", [128, 8, 128], BF16, 2)
        kvp = Pool(c, "kv", [128, 3072], BF16, 2)
        vxp = Pool(c, "vx", [128, 4, 257], BF16, 2)
        for v in vxp.tiles:
            pool_op(lambda v=v: nc.gpsimd.memset(v[:], 1.0), writes=[v])
        dgp = Pool(c, "dg", [128, 4, 128], F32, 2)
        e2p = Pool(c, "e2", [128, 4, 128], F32, 2)
        qpp = Pool(c, "qp", [128, 8, 128], BF16, 2)
        stp = Pool(c, "sT", [128, 128], BF16, 4)
        kpp = Pool(c, "kp", [128, 256], BF16, 4)
        hop = Pool(c, "ho", [128, D], F32, 2)
        smp = Pool(c, "sm", [128, 4], F32, 8)
        Cf = [c.sbuf("Cf%d" % h, [128, 2, 257], F32) for h in range(4)]
        Cb = [c.sbuf("Cb%d" % h, [128, 2, 257], BF16) for h in range(4)]
        hfp = Pool(c, "hfl", [128, D], F32, 2)
        hbp = Pool(c, "hab", [128, D], BF16, 2)
        stt = Pool(c, "bst", [128, 4, 8], F32, 2)
        hts = Pool(c, "hts", [128, KC, 128], BF16, 2)

        pre_n = 0
        if with_moe and l == 0:
            pstg = Pool(c, "pcs", [128, 4096], F32, 3)
            pstb = Pool(c, "pcb", [128, 4096], BF16, 3)
            PRE["gen"] = precast_units(pstg, pstb)
            total_units = 2 * NEXP * NFQ * 3
            pre_n = -(-total_units // (2 * NTT))
        for d in range(2):
            for h in range(4):
                dve(lambda h=h: nc.vector.memset(Cf[h][:], 0.0), writes=[Cf[h]])
                dve(lambda h=h: nc.vector.memset(Cb[h][:], 0.0), writes=[Cb[h]])
            order = range(NTT) if d == 0 else range(NTT - 1, -1, -1)
            msk = mask_f if d == 0 else mask_b
            for ch in order:
                t0 = ch * 128
                first_of_seg = (ch % CPS == 0) if d == 0 else (ch % CPS == CPS - 1)
                is_first = (ch == 0) if d == 0 else (ch == NTT - 1)
                if first_of_seg and not is_first:
                    for h in range(4):
                        dve(lambda h=h: nc.vector.tensor_scalar(out=Cf[h][:], in0=Cf[h][:], scalar1=keep[:, 0:1], scalar2=None, op0=ALU.mult), [Cf[h], keep], [Cf[h]])
                        act(lambda h=h: nc.scalar.copy(out=Cb[h][:], in_=Cf[h][:]), [Cf[h]], [Cb[h]])
                for _ in range(pre_n):
                    if PRE["gen"] is not None and next(PRE["gen"], "done") == "done":
                        PRE["gen"] = None
                qT = qtp.next(); kT = ktp.next(); kv = kvp.next(); vx = vxp.next()
                c.dma("sp", qT[:], QKT.t[0:8, :, t0:t0 + 128].rearrange("k p t -> p k t"), reads=[QKT], writes=[qT])
                c.dma("sp", kT[:], QKT.t[8:16, :, t0:t0 + 128].rearrange("k p t -> p k t"), reads=[QKT], writes=[kT])
                c.dma("sp", kv[:], KVO.t[t0:t0 + 128, :], reads=[KVO], writes=[kv])
                dve(lambda: nc.vector.tensor_copy(out=vx[:, :, 0:256], in_=kv[:, 1024:2048].rearrange("p (h e) -> p h e", h=4)), [kv], [vx])
                dg = dgp.next()
                for h in range(4):
                    dve(lambda h=h: nc.vector.tensor_scalar(out=dg[:, h, :], in0=ident_f[:], scalar1=BC[:, ch, d * 4 + h:d * 4 + h + 1], scalar2=None, op0=ALU.mult),
                        [ident_f, BC], [dg])
                ps = PSF.next()
                c.op("pe", lambda: nc.tensor.matmul(ps[:], lhsT=ones_f[:], rhs=dg[:].rearrange("p h i -> p (h i)"), start=True, stop=True), [ones_f, dg], [ps])
                e2 = e2p.next()
                act(lambda: nc.scalar.activation(out=e2[:].rearrange("p h i -> p (h i)"), in_=ps[:], func=AF.Exp), [ps], [e2])
                qp = qpp.next()
                for h in range(4):
                    for dc in range(2):
                        dve(lambda h=h, dc=dc: nc.vector.tensor_tensor(out=qp[:, 2 * h + dc, :], in0=qT[:, 2 * h + dc, :], in1=e2[:, h, :], op=ALU.mult), [qT, e2], [qp])
                ho = hop.next()
                elast = 127 if d == 0 else 0
                for h in range(4):
                    col = d * 4 + h
                    psA = PSF.next()
                    mm_group(psA, psA[:, 0:128], [(kT[:, 2 * h + dc, :], qp[:, 2 * h + dc, :]) for dc in range(2)], [kT, qp])
                    sT = stp.next()
                    dve(lambda: nc.vector.scalar_tensor_tensor(out=sT[:], in0=psA[:, 0:128], scalar=E1[:, ch, col:col + 1], in1=msk[:], op0=ALU.mult, op1=ALU.mult),
                        [psA, E1, msk], [sT])
                    kp = kpp.next()
                    act(lambda: nc.scalar.activation(out=kp[:], in_=kv[:, h * 256:(h + 1) * 256], func=AF.Copy, scale=E1[:, ch, col:col + 1]), [kv, E1], [kp])
                    psN = PSF.next()
                    mm_group(psN, psN[:, 0:257], [(sT[:], vx[:, h, :])] + [(qp[:, 2 * h + dc, :], Cb[h][:, dc, :]) for dc in range(2)], [sT, vx, qp, Cb[h]])
                    sm = smp.next()
                    act(lambda: nc.scalar.activation(out=sm[:, 0:1], in_=psN[:, 256:257], func=AF.Abs), [psN], [sm])
                    dve(lambda: nc.vector.tensor_scalar(out=sm[:, 0:1], in0=sm[:, 0:1], scalar1=1.0, scalar2=None, op0=ALU.max), [sm], [sm])
                    dve(lambda: nc.vector.reciprocal(out=sm[:, 1:2], in_=sm[:, 0:1]), [sm], [sm])
                    act(lambda: nc.scalar.activation(out=ho[:, h * 256:(h + 1) * 256], in_=psN[:, 0:256], func=AF.Copy, scale=sm[:, 1:2]), [psN, sm], [ho])
                    for dc in range(2):
                        psC = PSF.next()
                        mm_group(psC, psC[:, 0:257], [(kp[:, dc * 128:(dc + 1) * 128], vx[:, h, :])], [kp, vx])
                        dve(lambda: nc.vector.tensor_tensor(out=Cf[h][:, dc, :], in0=psC[:, 0:257], in1=Cf[h][:, dc, :], op=ALU.add), [psC, Cf[h]], [Cf[h]])
                    dve(lambda: nc.vector.tensor_scalar(out=Cf[h][:], in0=Cf[h][:], scalar1=e2[:, h, elast:elast + 1], scalar2=None, op0=ALU.mult), [Cf[h], e2], [Cf[h]])
                    act(lambda: nc.scalar.copy(out=Cb[h][:], in_=Cf[h][:]), [Cf[h]], [Cb[h]])
                if d == 0:
                    c.dma("act", HF.t[t0:t0 + 128, :], ho[:], reads=[ho], writes=[HF])
                else:
                    hf = hfp.next()
                    c.dma("sp", hf[:], HF.t[t0:t0 + 128, :], reads=[HF], writes=[hf])
                    dve(lambda: nc.vector.tensor_tensor(out=ho[:], in0=ho[:], in1=hf[:], op=ALU.add), [ho, hf], [ho])
                    st = stt.next()
                    for h in range(4):
                        dve(lambda h=h: nc.vector.bn_stats(out=st[:, h, 0:6], in_=ho[:, h * 256:(h + 1) * 256]), [ho], [st])
                        dve(lambda h=h: nc.vector.bn_aggr(out=st[:, h, 6:8], in_=st[:, h, 0:6]), [st], [st])
                        act(lambda h=h: nc.scalar.activation(out=st[:, h, 7:8], in_=st[:, h, 7:8], func=AF.Sqrt, bias=EPS, scale=1.0), [st], [st])
                        dve(lambda h=h: nc.vector.reciprocal(out=st[:, h, 7:8], in_=st[:, h, 7:8]), [st], [st])
                        dve(lambda h=h: nc.vector.tensor_scalar(out=ho[:, h * 256:(h + 1) * 256], in0=ho[:, h * 256:(h + 1) * 256], scalar1=st[:, h, 6:7],
                                                                scalar2=st[:, h, 7:8], op0=ALU.subtract, op1=ALU.mult), [ho, st], [ho])
                    dve(lambda: nc.vector.tensor_tensor(out=ho[:], in0=ho[:], in1=nwb[:], op=ALU.mult), [ho, nwb], [ho])
                    hb = hbp.next()
                    dve(lambda: nc.vector.tensor_tensor(out=hb[:], in0=ho[:], in1=kv[:, 2048:3072], op=ALU.mult), [ho, kv], [hb])
                    pb = PSB.next()
                    for kc in range(KC):
                        c.op("pe", lambda kc=kc: nc.tensor.transpose(pb[:, kc * 128:(kc + 1) * 128], hb[:, kc * 128:(kc + 1) * 128], ident_b[:]),
                             reads=[hb, ident_b], writes=[pb], inc=(kc == KC - 1))
                    ht = hts.next()
                    act(lambda: nc.scalar.copy(out=ht[:].rearrange("p k t -> p (k t)"), in_=pb[:]), [pb], [ht])
                    c.dma("act", HT[0].t[:, :, t0:t0 + 128].rearrange("k p t -> p k t"), ht[:], reads=[ht], writes=[HT[0]])
        while PRE["gen"] is not None:
            if next(PRE["gen"], "done") == "done":
                PRE["gen"] = None
        c.pop()


    def phase_na_proj(l):
        c.push()
        Wqk = c.sbuf("nWqk", [128, KC, 2048], BF16)
        Wv = c.sbuf("nWv", [128, KC, 1024], BF16)
        load_w(Wqk, l, OFF["bq"], 2048)
        load_w(Wv, l, OFF["bv"], 1024)
        bqk = c.sbuf("nbqk", [128, 16], F32)
        load_bias_fm(bqk, l, OFF["bq"], 16)
        brow = c.sbuf("nbrow", [128, 1024], F32)
        load_bias_row(brow, l, OFF["bv"], 1024)
        xbp = Pool(c, "nxb", [128, KC, 512], BF16, 2)
        stq = Pool(c, "nstq", [128, 16, 512], BF16, 2)
        stv = Pool(c, "nstv", [128, 1024], BF16, 2)
        for blk in range(NBLK):
            xb = xbp.next()
            load_xmblk(xb, blk)
            sq = stq.next()
            for ft in range(16):
                ps = PSF.next()
                mm_group(ps, ps[:], [(Wqk[:, kc, ft * 128:(ft + 1) * 128], xb[:, kc, :]) for kc in range(KC)], [Wqk, xb])
                sc = 0.125 if ft < 8 else 1.0
                dve(lambda: nc.vector.tensor_scalar(out=sq[:, ft, :], in0=ps[:], scalar1=bqk[:, ft:ft + 1], scalar2=sc, op0=ALU.add, op1=ALU.mult),
                    [ps, bqk], [sq])
            c.dma("act", QKT.t[:, :, blk * 512:(blk + 1) * 512].rearrange("k p t -> p k t"), sq[:], reads=[sq], writes=[QKT])
            for t4 in range(4):
                tt = blk * 4 + t4
                sv = stv.next()
                for nh in range(2):
                    ps = PSF.next()
                    mm_group(ps, ps[:], [(xb[:, kc, t4 * 128:(t4 + 1) * 128], Wv[:, kc, nh * 512:(nh + 1) * 512]) for kc in range(KC)], [Wv, xb])
                    dve(lambda: nc.vector.tensor_tensor(out=sv[:, nh * 512:(nh + 1) * 512], in0=ps[:], in1=brow[:, nh * 512:(nh + 1) * 512], op=ALU.add),
                        [ps, brow], [sv])
                c.dma("act", NV.t[tt * 128:(tt + 1) * 128, :], sv[:], reads=[sv], writes=[NV])
        c.pop()

    def phase_na_attn(l):
        c.push()
        T2 = c.sbuf("T2", [128, 16, 16, 64], BF16)
        tmpb = Pool(c, "btmp", [128, 4096], F32, 2)
        for part in range(4):
            tb = tmpb.next()
            c.dma("sp", tb[:], btab.t[l, :, part * 4096:(part + 1) * 4096], reads=[btab], writes=[tb])
            act(lambda: nc.scalar.activation(out=T2[:, part * 4:(part + 1) * 4, :, :].rearrange("p h x q -> p (h x q)"), in_=tb[:], func=AF.Exp), [tb], [T2])
        rvs = c.sbuf("rvs", [128, ROWS * 8], F32)
        c.dma("sp", rvs[:], rv_in.t, reads=[rv_in], writes=[rvs])
        NR = 10
        PSN = Pool(c, "psn", [128, 512], F32, 2, psum=True)
        PSN.tiles.append(PSL.tiles[0])
        kring = [c.sbuf("kr%d" % i, [128, 8, 128], BF16) for i in range(NR)]
        vring = [c.sbuf("vr%d" % i, [128, 1024], BF16) for i in range(NR)]
        loaded = {}
        qrp = Pool(c, "qrow", [128, 8, 2, 64], BF16, 3)
        for q_ in qrp.tiles:
            dve(lambda q_=q_: nc.vector.memset(q_[:], 0.0), [], [q_])
        pep = Pool(c, "pexp", [128, 512], BF16, 3)
        ptp = Pool(c, "ptall", [128, 8, 8, 64], BF16, 3)
        recp = Pool(c, "nrec", [64, 512], F32, 3)
        outp = Pool(c, "nout", [64, 8, 64], BF16, 3)
        nch = ROWS // 2
        rvP = _rv_table(ROWS, True); rvS = _rv_table(ROWS, False)
        for R in range(ROWS):
            cs = min(max((R - 7) // 2, 0), nch - 8)
            valid = [s_ for s_ in range(8) if 0 <= 2 * (cs + s_) - R + 8 <= 15
                     and (rvP[:, R * 8 + s_].any() or rvS[:, R * 8 + s_].any())]
            s_lo, s_hi = valid[0], valid[-1]
            assert valid == list(range(s_lo, s_hi + 1))
            ns = len(valid)
            x_lo = 2 * (cs + s_lo) - R + 8
            need_rv = not (rvP[:, R * 8 + s_lo:R * 8 + s_hi + 1].all() and rvS[:, R * 8 + s_lo:R * 8 + s_hi + 1].all())
            for s_ in valid:
                ch = cs + s_
                if loaded.get(ch % NR) != ch:
                    c.dma("sp", kring[ch % NR][:], QKT.t[8:16, :, ch * 128:(ch + 1) * 128].rearrange("k p t -> p k t"), reads=[QKT], writes=[kring[ch % NR]])
                    c.dma("sp", vring[ch % NR][:], NV.t[ch * 128:(ch + 1) * 128, :], reads=[NV], writes=[vring[ch % NR]])
                    loaded[ch % NR] = ch
            qr = qrp.next()
            c.dma("sp", qr[0:64, :, 0, :], QKT.t[0:8, 0:64, R * 64:(R + 1) * 64].rearrange("k p t -> p k t"), reads=[QKT], writes=[qr])
            c.dma("sp", qr[64:128, :, 1, :], QKT.t[0:8, 64:128, R * 64:(R + 1) * 64].rearrange("k p t -> p k t"), reads=[QKT], writes=[qr])
            groups = [list(range(g0, min(g0 + 4, ns))) for g0 in range(0, ns, 4)]
            for hh in range(2):
                pt = ptp.next()
                psnum = PSN.next()
                for hpl in range(4):
                    ft = hh * 4 + hpl
                    for grp in groups:
                        g = len(grp)
                        psS = PSF.next()
                        for gi, si in enumerate(grp):
                            kt = kring[(cs + valid[si]) % NR]
                            c.op("pe", lambda: nc.tensor.matmul(psS[:, gi * 128:(gi + 1) * 128], lhsT=kt[:, ft, :], rhs=qr[:, ft, :, :].rearrange("p two q -> p (two q)"), start=True, stop=True),
                                 reads=[kt, qr], writes=[psS], inc=(gi == g - 1))
                        pe_t = pep.next()
                        act(lambda: nc.scalar.activation(out=pe_t[:, 0:g * 128], in_=psS[:, 0:g * 128], func=AF.Exp), [psS], [pe_t])
                        xs0 = x_lo + 2 * grp[0]
                        dve(lambda: nc.vector.tensor_tensor(out=pt[:, 2 * hpl:2 * hpl + 2, grp[0]:grp[0] + g, :].rearrange("p two s q -> p s two q"),
                                                            in0=pe_t[:, 0:g * 128].rearrange("p (s two q) -> p s two q", two=2, q=64),
                                                            in1=T2[:, 2 * ft:2 * ft + 2, xs0:xs0 + 2 * g - 1:2, :].rearrange("p two s q -> p s two q"), op=ALU.mult), [pe_t, T2], [pt])
                    if need_rv:
                        for two in range(2):
                            hl = 2 * hpl + two
                            dve(lambda: nc.vector.tensor_tensor(out=pt[:, hl, 0:ns, :], in0=pt[:, hl, 0:ns, :],
                                                                in1=rvs[:, R * 8 + s_lo:R * 8 + s_hi + 1].unsqueeze(2).to_broadcast([128, ns, 64]), op=ALU.mult), [pt, rvs], [pt])
                    for two in range(2):
                        hl = 2 * hpl + two
                        h = hh * 8 + hl
                        for si, s_ in enumerate(valid):
                            vt = vring[(cs + s_) % NR]
                            c.op("pe", lambda: nc.tensor.matmul(psnum[0:64, hl * 64:(hl + 1) * 64], lhsT=vt[:, h * 64:(h + 1) * 64], rhs=pt[:, hl, si, :], start=(si == 0), stop=(si == ns - 1)),
                                 reads=[vt, pt], writes=[psnum], inc=(si == ns - 1))
                psden = PSF.next()
                for si in range(ns):
                    c.op("pe", lambda: nc.tensor.matmul(psden[0:64, :], lhsT=ones_b[:, 0:64], rhs=pt[:, :, si, :], start=(si == 0), stop=(si == ns - 1)),
                         reads=[ones_b, pt], writes=[psden], inc=(si == ns - 1))
                rec = recp.next()
                dve(lambda: nc.vector.reciprocal(out=rec[:], in_=psden[0:64, :]), [psden], [rec])
                ot = outp.next()
                dve(lambda: nc.vector.tensor_tensor(out=ot[:].rearrange("p h q -> p (h q)"), in0=psnum[0:64, :], in1=rec[:], op=ALU.mult), [psnum, rec], [ot])
                c.dma("act", HT[1].t[hh * 4:(hh + 1) * 4, :, R * 64:(R + 1) * 64].rearrange("f (two d) t -> d (f two) t", two=2), ot[:], reads=[ot], writes=[HT[1]])
        c.pop()

    def phase_lru_proj(l):
        c.push()
        W = c.sbuf("lW", [128, KC, 2048], BF16)
        load_w(W, l, OFF["cx"], 2048)
        bfm = c.sbuf("lbfm", [128, 16], F32)
        load_bias_fm(bfm, l, OFF["cx"], 16)
        xbp = Pool(c, "lxb", [128, KC, 512], BF16, 2)
        stp_ = Pool(c, "lst", [128, 16, 512], F32, 2)
        for blk in range(NBLK):
            xb = xbp.next()
            load_xmblk(xb, blk)
            st = stp_.next()
            for ft in range(16):
                ps = PSF.next()
                mm_group(ps, ps[:], [(W[:, kc, ft * 128:(ft + 1) * 128], xb[:, kc, :]) for kc in range(KC)], [W, xb])
                if ft % 2 == 0:
                    act(lambda: nc.scalar.activation(out=st[:, ft, :], in_=ps[:], func=AF.Identity, bias=bfm[:, ft:ft + 1], scale=1.0), [ps, bfm], [st])
                else:
                    dve(lambda: nc.vector.tensor_scalar(out=st[:, ft, :], in0=ps[:], scalar1=bfm[:, ft:ft + 1], scalar2=None, op0=ALU.add), [ps, bfm], [st])
            c.dma("act", CXY.t[:, :, blk * 512:(blk + 1) * 512].rearrange("k p t -> p k t"), st[:], reads=[st], writes=[CXY])
        c.pop()

    def phase_lru_scan(l):
        c.push()
        cw = c.sbuf("cw", [128, 8, 4], F32)
        cbv = c.sbuf("cbv", [128, 8], F32)
        gb = c.sbuf("gb", [128, 4, 8], F32)
        cL = c.sbuf("cL", [128, 2, 8], F32)
        ncw = c.sbuf("ncw", [128, 8, 4], F32)
        with nc.allow_non_contiguous_dma(reason="tiny per-channel params"):
            for j in range(4):
                c.dma("sp", cw[:, :, j], conv_w.t[l, j, :].rearrange("(f p) -> p f", p=128), reads=[conv_w], writes=[cw])
            c.dma("sp", cbv[:], conv_b.t[l, :].rearrange("(f p) -> p f", p=128), reads=[conv_b], writes=[cbv])
            for d in range(2):
                c.dma("sp", gb[:, d * 2 + 0, :], lru_ba.t[l, d, :].rearrange("(f p) -> p f", p=128), reads=[lru_ba], writes=[gb])
                c.dma("sp", gb[:, d * 2 + 1, :], lru_bx.t[l, d, :].rearrange("(f p) -> p f", p=128), reads=[lru_bx], writes=[gb])
                c.dma("sp", cL[:, d, :], lru_L.t[l, d, :].rearrange("(f p) -> p f", p=128), reads=[lru_L], writes=[cL])
        act(lambda: nc.scalar.activation(out=cL[:], in_=cL[:], func=AF.Exp, scale=-1.0), [cL], [cL])
        act(lambda: nc.scalar.activation(out=cL[:], in_=cL[:], func=AF.Ln, bias=1.0, scale=1.0), [cL], [cL])
        dve(lambda: nc.vector.tensor_scalar(out=cL[:], in0=cL[:], scalar1=-8.0, scalar2=None, op0=ALU.mult), [cL], [cL])
        dve(lambda: nc.vector.tensor_scalar(out=ncw[:], in0=cw[:], scalar1=nkeep[:, 0:1], scalar2=-1.0, op0=ALU.mult, op1=ALU.mult), [cw, nkeep], [ncw])
        bd = c.sbuf("bd", [128, 4, 8, 128], BF16)
        for g in range(4):
            c.dma("pool", bd[:, g, :, :], lru_bd.t[l, g].rearrange("f p j -> p f j"), reads=[lru_bd], writes=[bd])
        cxp = Pool(c, "cxhf", [128, Tn], F32, 1)
        xcp = Pool(c, "xc", [128, Tn], F32, 1)
        xbp = Pool(c, "xcb", [128, Tn], BF16, 1)
        Ap = Pool(c, "lA", [128, TSEG], F32, 2)
        Bp = Pool(c, "lB", [128, TSEG], F32, 2)
        hbp = Pool(c, "lhb", [128, TSEG], F32, 1)
        cyp = Pool(c, "lcy", [128, TSEG], F32, 2)
        t5 = Pool(c, "lt", [128, 512], F32, 6)
        car = Pool(c, "lcar", [128, 1], F32, 4)
        obp = Pool(c, "lob", [128, TSEG], BF16, 2)
        g1p = Pool(c, "lg1", [128, TSEG], F32, 1)
        for ft in range(8):
            cx = cxp.next(); xc = xcp.next(); xcb = xbp.next()
            c.dma("sp", cx[:], CXY.t[ft], reads=[CXY], writes=[cx])
            w = lambda j: cw[:, ft, j:j + 1]
            dve(lambda: nc.vector.tensor_scalar(out=xc[:], in0=cx[:], scalar1=w(2), scalar2=cbv[:, ft:ft + 1], op0=ALU.mult, op1=ALU.add), [cx, cw, cbv], [xc])
            dve(lambda: nc.vector.scalar_tensor_tensor(out=xc[:, 1:Tn], in0=cx[:, 0:Tn - 1], scalar=w(1), in1=xc[:, 1:Tn], op0=ALU.mult, op1=ALU.add), [cx, cw, xc], [xc])
            dve(lambda: nc.vector.scalar_tensor_tensor(out=xc[:, 2:Tn], in0=cx[:, 0:Tn - 2], scalar=w(0), in1=xc[:, 2:Tn], op0=ALU.mult, op1=ALU.add), [cx, cw, xc], [xc])
            dve(lambda: nc.vector.scalar_tensor_tensor(out=xc[:, 0:Tn - 1], in0=cx[:, 1:Tn], scalar=w(3), in1=xc[:, 0:Tn - 1], op0=ALU.mult, op1=ALU.add), [cx, cw, xc], [xc])
            nw_ = lambda j: ncw[:, ft, j:j + 1]
            for sgi in range(1, NSEG):
                b0 = sgi * TSEG
                for (dst_, src_, j) in ((b0, b0 - 1, 1), (b0, b0 - 2, 0), (b0 + 1, b0 - 1, 0), (b0 - 1, b0, 3)):
                    dve(lambda: nc.vector.scalar_tensor_tensor(out=xc[:, dst_:dst_ + 1], in0=cx[:, src_:src_ + 1], scalar=nw_(j), in1=xc[:, dst_:dst_ + 1],
                                                               op0=ALU.mult, op1=ALU.add), [cx, ncw, xc], [xc])
            act(lambda: nc.scalar.copy(out=xcb[:], in_=xc[:]), [xc], [xcb])
            hf = cx
            prev = None
            for d in range(2):
                segs = range(NSEG) if d == 0 else range(NSEG - 1, -1, -1)
                prev = None
                for sg in segs:
                    A = Ap.next(); Bt = Bp.next()
                    for b4 in range(TSEG // 512):
                        t0 = sg * TSEG + b4 * 512
                        lo_, hi_ = b4 * 512, (b4 + 1) * 512
                        psr = PSF.next()
                        mm_group(psr, psr[:], [(bd[:, d * 2 + 0, ft, :], xcb[:, t0:t0 + 512])], [bd, xcb])
                        psi = PSF.next()
                        mm_group(psi, psi[:], [(bd[:, d * 2 + 1, ft, :], xcb[:, t0:t0 + 512])], [bd, xcb])
                        r_ = t5.next(); i_ = t5.next(); u_ = t5.next()
                        act(lambda: nc.scalar.activation(out=r_[:], in_=psr[:], func=AF.Sigmoid, bias=gb[:, d * 2 + 0, ft:ft + 1], scale=1.0), [psr, gb], [r_])
                        act(lambda: nc.scalar.activation(out=i_[:], in_=psi[:], func=AF.Sigmoid, bias=gb[:, d * 2 + 1, ft:ft + 1], scale=1.0), [psi, gb], [i_])
                        act(lambda: nc.scalar.activation(out=A[:, lo_:hi_], in_=r_[:], func=AF.Exp, scale=cL[:, d, ft:ft + 1]), [r_, cL], [A])
                        dve(lambda: nc.vector.tensor_tensor(out=u_[:], in0=A[:, lo_:hi_], in1=A[:, lo_:hi_], op=ALU.mult), [A], [u_])
                        dve(lambda: nc.vector.tensor_scalar(out=u_[:], in0=u_[:], scalar1=-1.0, scalar2=1.0, op0=ALU.mult, op1=ALU.add), [u_], [u_])
                        act(lambda: nc.scalar.activation(out=u_[:], in_=u_[:], func=AF.Sqrt), [u_], [u_])
                        dve(lambda: nc.vector.tensor_tensor(out=i_[:], in0=i_[:], in1=xc[:, t0:t0 + 512], op=ALU.mult), [i_, xc], [i_])
                        dve(lambda: nc.vector.tensor_tensor(out=Bt[:, lo_:hi_], in0=i_[:], in1=u_[:], op=ALU.mult), [i_, u_], [Bt])
                    s0, s1 = sg * TSEG, (sg + 1) * TSEG
                    if prev is None:
                        init = 0.0
                        rd_init = []
                    else:
                        cr = car.next()
                        dve(lambda: nc.vector.tensor_scalar(out=cr[:], in0=prev[0], scalar1=keep[:, 0:1], scalar2=None, op0=ALU.mult), [prev[1], keep], [cr])
                        init = cr[:, 0:1]
                        rd_init = [cr]
                    if d == 0:
                        dve(lambda: nc.vector.tensor_tensor_scan(out=hf[:, s0:s1], data0=A[:], data1=Bt[:], initial=init, op0=ALU.mult, op1=ALU.add),
                            [A, Bt] + rd_init, [hf])
                        prev = (hf[:, s1 - 1:s1], hf)
                    else:
                        hb = hbp.next()
                        dve(lambda: nc.vector.tensor_tensor_scan(out=hb[:, ::-1], data0=A[:, ::-1], data1=Bt[:, ::-1], initial=init, op0=ALU.mult, op1=ALU.add),
                            [A, Bt] + rd_init, [hb])
                        cr2 = car.next()
                        dve(lambda: nc.vector.tensor_copy(out=cr2[:], in_=hb[:, 0:1]), [hb], [cr2])
                        prev = (cr2[:, 0:1], cr2)
                        dve(lambda: nc.vector.tensor_tensor(out=hb[:], in0=hb[:], in1=hf[:, s0:s1], op=ALU.add), [hb, hf], [hb])
                        cy = cyp.next()
                        c.dma("sp", cy[:], CXY.t[8 + ft, :, s0:s1], reads=[CXY], writes=[cy])
                        g1 = g1p.next()
                        act(lambda: nc.scalar.activation(out=g1[:], in_=cy[:], func=AF.Square), [cy], [g1])
                        dve(lambda: nc.vector.tensor_scalar(out=g1[:], in0=g1[:], scalar1=0.044715, scalar2=1.0, op0=ALU.mult, op1=ALU.add), [g1], [g1])
                        dve(lambda: nc.vector.tensor_tensor(out=g1[:], in0=g1[:], in1=cy[:], op=ALU.mult), [g1, cy], [g1])
                        act(lambda: nc.scalar.activation(out=g1[:], in_=g1[:], func=AF.Sigmoid, scale=1.5957691216057308), [g1], [g1])
                        dve(lambda: nc.vector.tensor_tensor(out=g1[:], in0=g1[:], in1=cy[:], op=ALU.mult), [g1, cy], [g1])
                        ob = obp.next()
                        dve(lambda: nc.vector.tensor_tensor(out=ob[:], in0=g1[:], in1=hb[:], op=ALU.mult), [g1, hb], [ob])
                        c.dma("act", HT[2].t[ft, :, s0:s1], ob[:], reads=[ob], writes=[HT[2]])
        c.pop()

    def phase_merge(l, src, dst):
        c.push()
        Wmg = c.sbuf("Wmg", [128, KC, 3072], BF16)
        load_w(Wmg, l, OFF["mg"], 3072)
        Wb = [c.sbuf("Wbr%d" % i, [128, KC, D], BF16) for i in range(3)]
        for i in range(3):
            c.dma("pool", Wb[i][:], w_br[i].t[l].rearrange("(k p) n -> p k n", p=128), reads=[w_br[i]], writes=[Wb[i]])
        Wo = c.sbuf("Wo", [128, KC, D], BF16)
        c.dma("pool", Wo[:], w_out.t[l].rearrange("(k p) n -> p k n", p=128), reads=[w_out], writes=[Wo])
        bmg = c.sbuf("bmg", [128, 24], F32)
        load_bias_fm(bmg, l, OFF["mg"], 24)
        g1b = c.sbuf("g1b", [128, D], F32)
        xb = c.sbuf("fxb", [128, KC, 512], BF16)
        hT = [c.sbuf("fhT%d" % i, [128, KC, 512], BF16) for i in range(3)]
        mt = c.sbuf("mt", [128, KC, 512], F32)
        mtb = c.sbuf("mtb", [128, KC, 512], BF16)
        sgp = Pool(c, "fsg", [128, 512], F32, 2)
        tmp = Pool(c, "ftmp", [128, 512], F32, 2)
        xtp = Pool(c, "fx", [128, D], F32, 2)
        cur_seg = -1
        for blk in range(NBLK):
            seg = (blk * 512) // TSEG
            if seg != cur_seg:
                c.dma("sp", g1b[:], MOD.t[l, seg:seg + 1, 2 * D:3 * D].to_broadcast([128, D]), reads=[MOD], writes=[g1b])
                cur_seg = seg
            load_xmblk(xb, blk)
            for i in range(3):
                c.dma("sp", hT[i][:], HT[i].t[:, :, blk * 512:(blk + 1) * 512].rearrange("k p t -> p k t"), reads=[HT[i]], writes=[hT[i]])
            for i in range(3):
                for ft in range(8):
                    psg = PSF.next()
                    col = i * 1024 + ft * 128
                    mm_group(psg, psg[:], [(Wmg[:, kc, col:col + 128], xb[:, kc, :]) for kc in range(KC)], [Wmg, xb])
                    sg_ = sgp.next()
                    act(lambda: nc.scalar.activation(out=sg_[:], in_=psg[:], func=AF.Sigmoid, bias=bmg[:, i * 8 + ft:i * 8 + ft + 1], scale=1.0), [psg, bmg], [sg_])
                    psb_ = PSF.next()
                    mm_group(psb_, psb_[:], [(Wb[i][:, kc, ft * 128:(ft + 1) * 128], hT[i][:, kc, :]) for kc in range(KC)], [Wb[i], hT[i]])
                    if i == 0:
                        dve(lambda: nc.vector.tensor_tensor(out=mt[:, ft, :], in0=psb_[:], in1=sg_[:], op=ALU.mult), [psb_, sg_], [mt])
                    else:
                        tm = tmp.next()
                        dve(lambda: nc.vector.tensor_tensor(out=tm[:], in0=psb_[:], in1=sg_[:], op=ALU.mult), [psb_, sg_], [tm])
                        if i == 1:
                            dve(lambda: nc.vector.tensor_tensor(out=mt[:, ft, :], in0=mt[:, ft, :], in1=tm[:], op=ALU.add), [mt, tm], [mt])
                        else:
                            dve(lambda: nc.vector.tensor_tensor(out=mtb[:, ft, :], in0=mt[:, ft, :], in1=tm[:], op=ALU.add), [mt, tm], [mtb])
            for t4 in range(4):
                tt = blk * 4 + t4
                xt = xtp.next()
                c.dma("sp", xt[:], src.t[tt * 128:(tt + 1) * 128, :], reads=[src], writes=[xt])
                for nh in range(2):
                    ps = PSF.next()
                    mm_group(ps, ps[:], [(mtb[:, kc, t4 * 128:(t4 + 1) * 128], Wo[:, kc, nh * 512:(nh + 1) * 512]) for kc in range(KC)], [mtb, Wo])
                    tm = tmp.next()
                    dve(lambda: nc.vector.tensor_tensor(out=tm[:], in0=ps[:], in1=g1b[:, nh * 512:(nh + 1) * 512], op=ALU.mult), [ps, g1b], [tm])
                    dve(lambda: nc.vector.tensor_tensor(out=xt[:, nh * 512:(nh + 1) * 512], in0=xt[:, nh * 512:(nh + 1) * 512], in1=tm[:], op=ALU.add), [xt, tm], [xt])
                c.dma("act", dst.t[tt * 128:(tt + 1) * 128, :], xt[:], reads=[xt], writes=[dst])
        c.pop()

    def phase_moe(l, src, dst, final):
        phase_norm(l, 1, src)
        c.push()
        wsel = c.sbuf("wsel", [128, NTT, NEXP], F32)
        c.push()
        Wr = c.sbuf("Wr", [128, KC, NEXP], BF16)
        c.dma("pool", Wr[:], w_router.t[l].rearrange("(k p) e -> p k e", p=128), reads=[w_router], writes=[Wr])
        brr = c.sbuf("brr", [128, NEXP], F32)
        c.dma("sp", brr[:], b_router.t[l:l + 1, :].to_broadcast([128, NEXP]), reads=[b_router], writes=[brr])
        AFs = c.sbuf("AFs", [128, NTT, NEXP], F32)
        xbp = Pool(c, "rxb", [128, KC, 512], BF16, 2)
        smp = Pool(c, "rsm", [128, 2], F32, 4)
        lgp = Pool(c, "rlg", [128, NEXP], F32, 4)
        for blk in range(NBLK):
            xb = xbp.next()
            load_xmblk(xb, blk)
            for t4 in range(4):
                tt = blk * 4 + t4
                ps = PSF.next()
                mm_group(ps, ps[:, 0:NEXP], [(xb[:, kc, t4 * 128:(t4 + 1) * 128], Wr[:, kc, :]) for kc in range(KC)], [xb, Wr])
                lg = lgp.next(); sm = smp.next()
                dve(lambda: nc.vector.tensor_tensor(out=lg[:], in0=ps[:, 0:NEXP], in1=brr[:], op=ALU.add), [ps, brr], [lg])
                act(lambda: nc.scalar.activation(out=lg[:], in_=lg[:], func=AF.Exp, accum_out=sm[:, 0:1]), [lg], [lg, sm])
                dve(lambda: nc.vector.reciprocal(out=sm[:, 1:2], in_=sm[:, 0:1]), [sm], [sm])
                dve(lambda: nc.vector.tensor_scalar(out=AFs[:, tt, :], in0=lg[:], scalar1=sm[:, 1:2], scalar2=None, op0=ALU.mult), [lg, sm], [AFs])
        c.dma("act", AFF.t.rearrange("(t p) e -> p t e", p=128), AFs[:], reads=[AFs], writes=[AFF])
        c.allgather(AFF, AFFALL, GROUPS)
        NF = 4 * Tn // 128
        AA = c.sbuf("AA", [128, NF, NEXP], F32)
        c.dma("sp", AA[:], AFFALL.t.rearrange("(p f) e -> p f e", p=128), reads=[AFFALL], writes=[AA])
        cmpT = c.sbuf("cmpT", [128, NF, NEXP], F32)
        lo = c.sbuf("blo", [128, NEXP], F32); hi = c.sbuf("bhi", [128, NEXP], F32); mid = c.sbuf("bmid", [128, NEXP], F32)
        cnt = c.sbuf("bcnt", [128, NEXP], F32); ge = c.sbuf("bge", [128, NEXP], F32); d1 = c.sbuf("bd1", [128, NEXP], F32)
        dve(lambda: nc.vector.memset(lo[:], 0.0), [], [lo])
        dve(lambda: nc.vector.memset(hi[:], 1.0), [], [hi])
        for it in range(30):
            dve(lambda: nc.vector.tensor_tensor(out=mid[:], in0=lo[:], in1=hi[:], op=ALU.add), [lo, hi], [mid])
            dve(lambda: nc.vector.tensor_scalar(out=mid[:], in0=mid[:], scalar1=0.5, scalar2=None, op0=ALU.mult), [mid], [mid])
            dve(lambda: nc.vector.tensor_tensor(out=cmpT[:], in0=AA[:], in1=mid[:].unsqueeze(1).to_broadcast([128, NF, NEXP]), op=ALU.is_gt), [AA, mid], [cmpT])
            dve(lambda: nc.vector.tensor_reduce(out=cnt[:], in_=cmpT[:].rearrange("p f e -> p e f"), axis=AX.X, op=ALU.add), [cmpT], [cnt])
            ps = PSF.next()
            c.op("pe", lambda: nc.tensor.matmul(ps[:, 0:NEXP], lhsT=ones_f[:], rhs=cnt[:], start=True, stop=True), [ones_f, cnt], [ps])
            dve(lambda: nc.vector.tensor_scalar(out=ge[:], in0=ps[:, 0:NEXP], scalar1=float(cap) - 0.5, scalar2=None, op0=ALU.is_ge), [ps], [ge])
            dve(lambda: nc.vector.tensor_tensor(out=d1[:], in0=mid[:], in1=lo[:], op=ALU.subtract), [mid, lo], [d1])
            dve(lambda: nc.vector.tensor_tensor(out=d1[:], in0=d1[:], in1=ge[:], op=ALU.mult), [d1, ge], [d1])
            dve(lambda: nc.vector.tensor_tensor(out=lo[:], in0=lo[:], in1=d1[:], op=ALU.add), [lo, d1], [lo])
            dve(lambda: nc.vector.tensor_tensor(out=d1[:], in0=hi[:], in1=mid[:], op=ALU.subtract), [hi, mid], [d1])
            dve(lambda: nc.vector.tensor_tensor(out=d1[:], in0=d1[:], in1=ge[:], op=ALU.mult), [d1, ge], [d1])
            dve(lambda: nc.vector.tensor_tensor(out=hi[:], in0=mid[:], in1=d1[:], op=ALU.add), [mid, d1], [hi])
        dve(lambda: nc.vector.tensor_tensor(out=wsel[:], in0=AFs[:], in1=lo[:].unsqueeze(1).to_broadcast([128, NTT, NEXP]), op=ALU.is_gt), [AFs, lo], [wsel])
        dve(lambda: nc.vector.tensor_tensor(out=wsel[:], in0=wsel[:], in1=AFs[:], op=ALU.mult), [wsel, AFs], [wsel])
        c.pop()
        if "WSEL" in dbg:
            o = T(nc.dram_tensor("dbg_WSEL%d" % l, [128, NTT * NEXP], F32, kind="ExternalOutput").ap(), Tok("dbgw"))
            c.dma("sp", o.t, wsel[:].rearrange("p t e -> p (t e)"), reads=[wsel], writes=[o])
            dbg_out["WSEL%d" % l] = o
        TB = min(2048, Tn)
        NTB = Tn // TB
        yacc = c.sbuf("yacc", [128, TB // 128, D], F32)
        xmb = c.sbuf("xmb", [128, KC, TB], BF16)
        wgp = Pool(c, "ewg", [128, KC, 512], BF16, 2)
        wup = Pool(c, "ewu", [128, KC, 512], BF16, 2)
        wdp = Pool(c, "ewd", [128, 4, D], BF16, 2)
        htp = Pool(c, "eh", [128, 4, 512], BF16, 2)
        slp = Pool(c, "esl", [128, 512], F32, 3)
        xtp = Pool(c, "ex", [128, D], F32, 2)
        g2b = c.sbuf("g2b", [128, D], F32)
        fwb = c.sbuf("fwb", [128, D], F32)
        junk = c.sbuf("ejunk", [128, D], BF16)
        ssp = Pool(c, "ess", [128, 2], F32, 4)
        if final:
            c.dma("sp", fwb[:], final_norm_w.t[0:1, :].to_broadcast([128, D]), reads=[final_norm_w], writes=[fwb])
        for tb in range(NTB):
            c.dma("sp", xmb[:], XMT.t[:, :, tb * TB:(tb + 1) * TB].rearrange("k p t -> p k t"), reads=[XMT], writes=[xmb])
            dve(lambda: nc.vector.memset(yacc[:], 0.0), [], [yacc])
            for e in range(NEXP):
                for fq in range(DEXP // 512):
                    wg = wgp.next(); wu = wup.next(); wd = wdp.next()
                    idx = (l * NEXP + e) * NFQ + fq
                    c.dma("sp", wg[:].rearrange("p k n -> p (k n)"), EGb.t[idx], reads=[EGb], writes=[wg])
                    c.dma("sp", wu[:].rearrange("p k n -> p (k n)"), EUb.t[idx], reads=[EUb], writes=[wu])
                    c.dma("sp", wd[:].rearrange("p k n -> p (k n)"), EDb.t[idx], reads=[EDb], writes=[wd])
                    for sb in range(TB // 512):
                        ht = htp.next()
                        for fi in range(4):
                            psg = PSF.next()
                            mm_group(psg, psg[:], [(wg[:, kc, fi * 128:(fi + 1) * 128], xmb[:, kc, sb * 512:(sb + 1) * 512]) for kc in range(KC)], [wg, xmb])
                            psu = PSF.next()
                            mm_group(psu, psu[:], [(wu[:, kc, fi * 128:(fi + 1) * 128], xmb[:, kc, sb * 512:(sb + 1) * 512]) for kc in range(KC)], [wu, xmb])
                            sl = slp.next()
                            act(lambda: nc.scalar.activation(out=sl[:], in_=psg[:], func=AF.Silu), [psg], [sl])
                            dve(lambda: nc.vector.tensor_tensor(out=ht[:, fi, :], in0=psu[:], in1=sl[:], op=ALU.mult), [psu, sl], [ht])
                        for t4 in range(4):
                            ttl = sb * 4 + t4
                            ttg = tb * (TB // 128) + ttl
                            for nh in range(2):
                                ps = PSF.next()
                                mm_group(ps, ps[:], [(ht[:, fi, t4 * 128:(t4 + 1) * 128], wd[:, fi, nh * 512:(nh + 1) * 512]) for fi in range(4)], [ht, wd])
                                dve(lambda: nc.vector.scalar_tensor_tensor(out=yacc[:, ttl, nh * 512:(nh + 1) * 512], in0=ps[:], scalar=wsel[:, ttg, e:e + 1],
                                                                           in1=yacc[:, ttl, nh * 512:(nh + 1) * 512], op0=ALU.mult, op1=ALU.add), [ps, wsel, yacc], [yacc])
            cur_seg = -1
            for ttl in range(TB // 128):
                ttg = tb * (TB // 128) + ttl
                seg = seg_of_tt(ttg)
                if seg != cur_seg:
                    c.dma("sp", g2b[:], MOD.t[l, seg:seg + 1, 5 * D:6 * D].to_broadcast([128, D]), reads=[MOD], writes=[g2b])
                    cur_seg = seg
                xt = xtp.next()
                c.dma("sp", xt[:], src.t[ttg * 128:(ttg + 1) * 128, :], reads=[src], writes=[xt])
                dve(lambda: nc.vector.tensor_tensor(out=yacc[:, ttl, :], in0=yacc[:, ttl, :], in1=g2b[:], op=ALU.mult), [yacc, g2b], [yacc])
                dve(lambda: nc.vector.tensor_tensor(out=xt[:], in0=xt[:], in1=yacc[:, ttl, :], op=ALU.add), [xt, yacc], [xt])
                if final:
                    ss = ssp.next()
                    act(lambda: nc.scalar.activation(out=junk[:], in_=xt[:], func=AF.Square, accum_out=ss[:, 0:1]), [xt], [junk, ss])
                    act(lambda: nc.scalar.activation(out=ss[:, 1:2], in_=ss[:, 0:1], func=AF.Sqrt, bias=EPS, scale=1.0 / D), [ss], [ss])
                    dve(lambda: nc.vector.reciprocal(out=ss[:, 1:2], in_=ss[:, 1:2]), [ss], [ss])
                    dve(lambda: nc.vector.scalar_tensor_tensor(out=xt[:], in0=xt[:], scalar=ss[:, 1:2], in1=fwb[:], op0=ALU.mult, op1=ALU.mult), [xt, ss, fwb], [xt])
                c.dma("act", dst.t[ttg * 128:(ttg + 1) * 128, :], xt[:], reads=[xt], writes=[dst])
        c.pop()

    src = x_in
    done = False
    for l in range(2):
        phase_norm(l, 0, src)
        if l == 0:
            dbg_dump("XMT", XMT, [KC, 128, Tn], BF16)
        if stop_after == "normA":
            done = True; break
        c.push()
        GA = c.sbuf("GA", [128, NTT, 16], F32)
        phase_mlstm_proj(l, GA)
        if l == 0:
            dbg_dump("QKT", QKT, [16, 128, Tn], BF16)
            dbg_dump("KVO", KVO, [Tn, 3 * D], BF16)
        if stop_after == "mproj":
            c.pop(); done = True; break
        phase_mlstm_scan(l, GA)
        c.pop()
        if l == 0:
            dbg_dump("HF", HF, [Tn, D], F32)
            dbg_dump("HAT", HT[0], [KC, 128, Tn], BF16)
        if stop_after == "mscan":
            done = True; break
        phase_na_proj(l)
        phase_na_attn(l)
        if l == 0:
            dbg_dump("HBT", HT[1], [KC, 128, Tn], BF16)
        if stop_after == "na":
            done = True; break
        phase_lru_proj(l)
        phase_lru_scan(l)
        if l == 0:
            dbg_dump("HCT", HT[2], [KC, 128, Tn], BF16)
        if stop_after == "lru":
            done = True; break
        phase_merge(l, src, X[0])
        if l == 0:
            dbg_dump("X0", X[0], [Tn, D], F32)
        if stop_after == "merge":
            done = True; break
        last = (l == 1)
        phase_moe(l, X[0], y_out if last else X[1], last)
        if l == 0:
            dbg_dump("X1", X[1], [Tn, D], F32)
        if stop_after == "moe":
            done = True; break
        src = X[1]

    if done:
        c.push()
        tp = Pool(c, "cpy", [128, D], F32, 2)
        for tt in range(NTT):
            t = tp.next()
            c.dma("sp", t[:], x_in.t[tt * 128:(tt + 1) * 128, :], reads=[x_in], writes=[t])
            c.dma("act", y_out.t[tt * 128:(tt + 1) * 128, :], t[:], reads=[t], writes=[y_out])
        c.pop()
    c.close()
    return nc, list(dbg_out.keys())


def _rv_table(rows, prompt):
    rv = np.zeros((128, rows, 8), np.float32)
    rseg = rows // NSEG
    for R in range(rows):
        cs = min(max((R - 7) // 2, 0), rows // 2 - 8)
        if prompt:
            rs = min(max(R - 4, 0), rows - 8)
        else:
            s, r = divmod(R, rseg)
            rs = s * rseg + min(max(r - 4, 0), rseg - 8)
        for sl in range(8):
            for rr in range(2):
                kr = 2 * (cs + sl) + rr
                if rs <= kr <= rs + 7:
                    rv[rr * 64:(rr + 1) * 64, R, sl] = 1.0
    return rv.reshape(128, rows * 8)


def _bias_table(rpb):
    q = np.arange(64)
    kc = np.arange(64)
    cs = np.clip(q - 8, 0, 48)
    col_in = (kc[None, :] >= cs[:, None]) & (kc[None, :] < cs[:, None] + 16)
    dc = np.clip(kc[None, :] - q[:, None], -15, 15) + 15
    out = np.full((2, 128, 16, 16, 64), NEGM, np.float32)
    for rr in range(2):
        for x in range(16):
            di = x - 1 + rr
            if di < 0 or di > 14:
                continue
            g = rpb[:, :, di, :][:, :, dc]
            g = np.where(col_in[None, None], g, NEGM)
            out[:, rr * 64:(rr + 1) * 64, :, x, :] = np.transpose(g, (0, 3, 1, 2))
    return np.ascontiguousarray(out.reshape(2, 128, 16 * 16 * 64))


def _lru_bd(wa, wx):
    out = np.zeros((2, 4, 8, 128, 128), np.float32)
    for l in range(2):
        for d in range(2):
            for gi, w in enumerate((wa, wx)):
                for ft in range(8):
                    for b in range(2):
                        out[l, d * 2 + gi, ft, b * 64:(b + 1) * 64, b * 64:(b + 1) * 64] = w[l, d, ft * 2 + b]
    return out


def prep_inputs(inp, Tn, with_moe=True):
    f = lambda a: np.ascontiguousarray(np.asarray(a, np.float32))
    xp = f(inp["x_prompt"]); xs = f(inp["x_sample"]); cp = f(inp["c_prompt"]); cs_ = f(inp["c_sample"])
    rows = Tn // 64
    names = ["norm1_w", "norm2_w", "w_mod", "b_mod", "w_in", "b_in", "mlstm_norm_w", "conv_w", "conv_b",
             "lru_ba", "lru_bx", "lru_L", "w_br_a", "w_br_b", "w_br_c", "w_out", "w_router", "b_router"]
    if with_moe:
        names += ["w_gate_e", "w_up_e", "w_down_e"]
    shared = {k: f(inp[k]) for k in names}
    shared["final_norm_w"] = f(inp["final_norm_w"]).reshape(1, D)
    shared["btab"] = _bias_table(f(inp["na_rpb"]))
    shared["lru_bd"] = _lru_bd(f(inp["lru_wa"]), f(inp["lru_wx"]))
    rvp = _rv_table(rows, True); rvs = _rv_table(rows, False)
    maps = []
    nps = xs.shape[0] // 4
    for i in range(8):
        if i < 4:
            x = xp[i].reshape(Tn, D)
            c4 = np.repeat(cp[i:i + 1], NSEG, axis=0)
            keep = np.ones((128, 1), np.float32); rv = rvp
        else:
            j = i - 4
            x = xs[j * nps:(j + 1) * nps].reshape(Tn, D)
            c4 = cs_[j * nps:(j + 1) * nps]
            keep = np.zeros((128, 1), np.float32); rv = rvs
        c4T = np.ascontiguousarray(c4.reshape(NSEG, KC, 128).transpose(2, 1, 0))
        m = dict(shared)
        m.update(x=np.ascontiguousarray(x), c4T=c4T, keep=keep, rv=rv)

        maps.append(m)
    return maps


_CACHE = {}


def run(inp, stop_after=None, dbg=()):
    B, S, _ = inp["x_prompt"].shape
    Tn = S
    assert inp["x_sample"].shape[0] * inp["x_sample"].shape[1] == 4 * Tn
    cap = 2 * (B * S) // NEXP
    dexp = inp["w_gate_e"].shape[-1]
    key = (Tn, cap, stop_after, tuple(dbg), dexp)
    if key not in _CACHE:
        _CACHE[key] = build(Tn, cap, stop_after, dbg, DEXP=dexp)
    nc, dnames = _CACHE[key]
    maps = prep_inputs(inp, Tn, with_moe=(stop_after is None or stop_after in ("moe",)))
    res = run_bass_kernel_spmd(nc, maps, core_ids=list(range(8)))
    ys = [r["y"] for r in res.results]
    yp = np.stack(ys[:4], 0).reshape(inp["x_prompt"].shape).astype(np.float32)
    ysm = np.concatenate(ys[4:], 0).reshape(inp["x_sample"].shape).astype(np.float32)
    return (yp, ysm), res.results


def kernel(**inputs):
    out, _ = run(inputs)
    return out
```

```python
import numpy as np
from contextlib import ExitStack
import concourse.bass as bass
import concourse.mybir as mybir
from concourse.bass_utils import run_bass_kernel_spmd

F32 = mybir.dt.float32
BF16 = mybir.dt.bfloat16
I32 = mybir.dt.int32
ALU = mybir.AluOpType
AF = mybir.ActivationFunctionType
AX = mybir.AxisListType

D = 1024
KC = 8
NSEG = 4
N_IN = 12304
OFF = dict(aq=0, ak=1024, av=2048, ao=3072, ag=4096, bq=4112, bk=5136, bv=6160, cx=7184, cy=8208, mg=9232)
NEXP = 16
DEXP = 2048
EPS = 1e-6
NEGM = -30000.0


class Tok:
    __slots__ = ("w", "r", "name")

    def __init__(self, name=""):
        self.w = None
        self.r = []
        self.name = name


class T:
    def __init__(self, t, tok):
        self.t = t
        self.tok = tok

    def __getitem__(self, k):
        return self.t[k]


class Ctx:
    ENG = ("pe", "act", "dve", "pool", "sp")

    def __init__(self, nc, n_dma_sems=32):
        self.nc = nc
        self.es = ExitStack()
        self.scopes = []
        self.eng = {"pe": nc.tensor, "act": nc.scalar, "dve": nc.vector, "pool": nc.gpsimd, "sp": nc.sync}
        self.sem = {}
        self.cnt = {}
        for e in self.ENG:
            self.sem[e] = self.es.enter_context(nc.semaphore("s_" + e))
            self.cnt[e] = 0
        self.dsem = []
        self.dcnt = []
        for i in range(n_dma_sems):
            self.dsem.append(self.es.enter_context(nc.semaphore("d%d" % i)))
            self.dcnt.append(0)
        self.dnext = 0
        self.ccsem = self.es.enter_context(nc.semaphore("ccsem"))
        self.cccnt = 0
        self.known = {e: {} for e in self.ENG}
        self.ninst = 0

    def _stack(self):
        return self.scopes[-1] if self.scopes else self.es

    def push(self):
        self.scopes.append(ExitStack())

    def pop(self):
        self.barrier()
        self.scopes.pop().close()

    def sbuf(self, name, shape, dtype):
        self.uid = getattr(self, "uid", 0) + 1
        t = self._stack().enter_context(self.nc.sbuf_tensor("sb%d_%s" % (self.uid, name), list(shape), dtype))
        return T(t, Tok(name))

    def psum(self, name, shape, dtype):
        self.uid = getattr(self, "uid", 0) + 1
        t = self._stack().enter_context(self.nc.psum_tensor("ps%d_%s" % (self.uid, name), list(shape), dtype))
        return T(t, Tok(name))

    def dram(self, name, shape, dtype, kind="Internal"):
        t = self.nc.dram_tensor(name, list(shape), dtype, kind=kind)
        return T(t.ap(), Tok(name))

    def _semobj(self, key):
        if key == "cc":
            return self.ccsem
        if isinstance(key, str):
            return self.sem[key]
        return self.dsem[key]

    def _wait(self, e, key, val):
        k = self.known[e]
        if k.get(key, 0) >= val:
            return
        self.eng[e].wait_ge(self._semobj(key), val)
        k[key] = val

    def _deps(self, e, reads, writes):
        need = {}

        def add(d, same_ok):
            key, val, de = d
            if de == e and same_ok:
                return
            if need.get(key, 0) < val:
                need[key] = val
        for t in reads:
            if t.w is not None:
                add(t.w, False)
        for t in writes:
            if t.w is not None:
                add(t.w, True)
            for d in t.r:
                add(d, True)
        for key, val in need.items():
            self._wait(e, key, val)

    def _mark(self, reads, writes, d):
        for t in writes:
            t.w = d
            t.r = []
        for t in reads:
            if t in writes:
                continue
            t.r = [x for x in t.r if x[0] != d[0]]
            t.r.append(d)

    @staticmethod
    def _toks(lst):
        out = []
        for x in lst:
            if x is None:
                continue
            out.append(x.tok if isinstance(x, T) else x)
        return out

    def op(self, e, fn, reads=(), writes=(), inc=True):
        reads = self._toks(reads)
        writes = self._toks(writes)
        self._deps(e, reads, writes)
        inst = fn()
        self.ninst += 1
        if inc:
            self.cnt[e] += 1
            inst.then_inc(self.sem[e], 1)
            val = self.cnt[e]
        else:
            val = self.cnt[e] + 1
        self._mark(reads, writes, (e, val, e))
        return inst

    def dma(self, q, out, in_, reads=(), writes=(), **kw):
        reads = self._toks(reads)
        writes = self._toks(writes)
        self._deps(q, reads, writes)
        i = self.dnext
        self.dnext = (self.dnext + 1) % len(self.dsem)
        if self.dcnt[i] > 0:
            self._wait(q, i, self.dcnt[i])
        inst = self.eng[q].dma_start(out=out, in_=in_, **kw)
        self.ninst += 1
        self.dcnt[i] += 16
        inst.then_inc(self.dsem[i], 16)
        self._mark(reads, writes, (i, self.dcnt[i], "dma"))
        return inst

    def allgather(self, in_t, out_t, groups):
        reads = [in_t.tok]
        writes = [out_t.tok]
        self._deps("pool", reads, writes)
        inst = self.nc.gpsimd.collective_compute("AllGather", op=ALU.bypass, replica_groups=groups,
                                                 ins=[in_t.t], outs=[out_t.t])
        self.cccnt += 1
        inst.then_inc(self.ccsem, 1)
        self._mark(reads, writes, ("cc", self.cccnt, "cc"))

    def barrier(self):
        for e in self.ENG:
            for x in self.ENG:
                if x != e and self.cnt[x] > 0:
                    self._wait(e, x, self.cnt[x])
            for i in range(len(self.dsem)):
                if self.dcnt[i] > 0:
                    self._wait(e, i, self.dcnt[i])
            if self.cccnt:
                self._wait(e, "cc", self.cccnt)

    def close(self):
        self.barrier()
        while self.scopes:
            self.scopes.pop().close()
        self.es.close()


class Pool:
    def __init__(self, c, name, shape, dtype, n, psum=False):
        self.tiles = [(c.psum if psum else c.sbuf)("%s%d" % (name, i), shape, dtype) for i in range(n)]
        self.i = 0

    def next(self):
        t = self.tiles[self.i]
        self.i = (self.i + 1) % len(self.tiles)
        return t


def build(Tn, cap, stop_after=None, dbg=(), DEXP=DEXP):
    with_moe = stop_after is None or stop_after in ("moe",)
    nc = bass.Bass("TRN2", target_bir_lowering=False)
    c = Ctx(nc)
    TSEG = Tn // NSEG
    NTT = Tn // 128
    NBLK = Tn // 512
    ROWS = Tn // 64
    CPS = TSEG // 128

    def din(name, shape, dt=F32):
        return T(nc.dram_tensor(name, list(shape), dt, kind="ExternalInput").ap(), Tok(name))

    x_in = din("x", [Tn, D])
    c4T = din("c4T", [128, KC, NSEG])
    keep_in = din("keep", [128, 1])
    rv_in = din("rv", [128, ROWS * 8])
    norm1_w = din("norm1_w", [2, D]); norm2_w = din("norm2_w", [2, D])
    GROUPS = [[0, 1, 2, 3], [4, 5, 6, 7]]
    b_mod = din("b_mod", [2, 6 * D])
    b_in = din("b_in", [2, N_IN])
    mlstm_norm_w = din("mlstm_norm_w", [2, D])
    btab = din("btab", [2, 128, 16 * 16 * 64])
    conv_w = din("conv_w", [2, 4, D]); conv_b = din("conv_b", [2, D])
    lru_bd = din("lru_bd", [2, 4, 8, 128, 128])
    lru_ba = din("lru_ba", [2, 2, D]); lru_bx = din("lru_bx", [2, 2, D]); lru_L = din("lru_L", [2, 2, D])
    w_router = din("w_router", [2, D, NEXP]); b_router = din("b_router", [2, NEXP])
    w_in = din("w_in", [2, D, N_IN])
    w_mod = din("w_mod", [2, D, 6 * D])
    w_br = [din("w_br_a", [2, D, D]), din("w_br_b", [2, D, D]), din("w_br_c", [2, D, D])]
    w_out = din("w_out", [2, D, D])
    if with_moe:
        EG = din("w_gate_e", [2, NEXP, D, DEXP]); EU = din("w_up_e", [2, NEXP, D, DEXP]); ED = din("w_down_e", [2, NEXP, DEXP, D])

        def expert_w(t_, l, e, nrows):
            return T(t_.t[l, e], t_.tok)
    final_norm_w = din("final_norm_w", [1, D])
    y_out = T(nc.dram_tensor("y", [Tn, D], F32, kind="ExternalOutput").ap(), Tok("y"))

    X = [c.dram("Xs0", [Tn, D], F32), c.dram("Xs1", [Tn, D], F32)]
    MOD = c.dram("MOD", [2, NSEG, 6 * D], F32)
    XMT = c.dram("XMT", [KC, 128, Tn], BF16)
    QKT = c.dram("QKT", [16, 128, Tn], BF16)
    KVO = c.dram("KVO", [Tn, 3 * D], BF16)
    HF = c.dram("HF", [Tn, D], F32)
    HT = [c.dram("HAT", [KC, 128, Tn], BF16), c.dram("HBT", [KC, 128, Tn], BF16), c.dram("HCT", [KC, 128, Tn], BF16)]
    NV = c.dram("NV", [Tn, D], BF16)
    CXY = c.dram("CXY", [16, 128, Tn], F32)
    AFF = c.dram("AFF", [Tn, NEXP], F32)
    AFFALL = c.dram("AFFALL", [4 * Tn, NEXP], F32)

    if with_moe:
        NFQ = DEXP // 512
        EGb = c.dram("EGb", [2 * NEXP * NFQ, 128, 4096], BF16)
        EUb = c.dram("EUb", [2 * NEXP * NFQ, 128, 4096], BF16)
        EDb = c.dram("EDb", [2 * NEXP * NFQ, 128, 4096], BF16)
    dbg_out = {}

    def dbg_dump(name, src_t, shape, dt=F32):
        if name in dbg:
            o = T(nc.dram_tensor("dbg_" + name, list(shape), dt, kind="ExternalOutput").ap(), Tok("dbg_" + name))
            c.dma("sp", o.t, src_t.t, reads=[src_t], writes=[o])
            dbg_out[name] = o

    ident_f = c.sbuf("ident_f", [128, 128], F32)
    ident_b = c.sbuf("ident_b", [128, 128], BF16)
    triu_f = c.sbuf("triu_f", [128, 128], F32)
    tril_f = c.sbuf("tril_f", [128, 128], F32)
    mask_f = c.sbuf("mask_f", [128, 128], BF16)
    mask_b = c.sbuf("mask_b", [128, 128], BF16)
    ones_f = c.sbuf("ones_f", [128, 128], F32)
    ones_b = c.sbuf("ones_b", [128, 128], BF16)
    keep = c.sbuf("keep", [128, 1], F32)
    nkeep = c.sbuf("nkeep", [128, 1], F32)

    def pool_op(fn, reads=(), writes=()):
        return c.op("pool", fn, reads, writes)

    def dve(fn, reads=(), writes=()):
        return c.op("dve", fn, reads, writes)

    def act(fn, reads=(), writes=()):
        return c.op("act", fn, reads, writes)

    pool_op(lambda: nc.gpsimd.memset(ones_f[:], 1.0), writes=[ones_f])
    pool_op(lambda: nc.gpsimd.memset(ones_b[:], 1.0), writes=[ones_b])
    pool_op(lambda: nc.gpsimd.affine_select(out=ident_f[:], in_=ones_f[:], pattern=[[-1, 128]], compare_op=ALU.is_equal,
                                            fill=0.0, base=0, channel_multiplier=1), reads=[ones_f], writes=[ident_f])
    pool_op(lambda: nc.gpsimd.affine_select(out=triu_f[:], in_=ones_f[:], pattern=[[1, 128]], compare_op=ALU.is_ge,
                                            fill=0.0, base=0, channel_multiplier=-1), reads=[ones_f], writes=[triu_f])
    pool_op(lambda: nc.gpsimd.affine_select(out=tril_f[:], in_=ones_f[:], pattern=[[-1, 128]], compare_op=ALU.is_ge,
                                            fill=0.0, base=0, channel_multiplier=1), reads=[ones_f], writes=[tril_f])
    dve(lambda: nc.vector.tensor_copy(out=ident_b[:], in_=ident_f[:]), [ident_f], [ident_b])
    dve(lambda: nc.vector.tensor_copy(out=mask_f[:], in_=triu_f[:]), [triu_f], [mask_f])
    dve(lambda: nc.vector.tensor_copy(out=mask_b[:], in_=tril_f[:]), [tril_f], [mask_b])
    c.dma("sp", keep[:], keep_in.t, writes=[keep])
    dve(lambda: nc.vector.tensor_scalar(out=nkeep[:], in0=keep[:], scalar1=-1.0, scalar2=1.0, op0=ALU.mult, op1=ALU.add),
        [keep], [nkeep])

    PSF = Pool(c, "psf", [128, 512], F32, 5, psum=True)
    PSL = Pool(c, "psl", [128, 512], F32, 1, psum=True)
    PSB = Pool(c, "psb", [128, 1024], BF16, 2, psum=True)

    def mm_group(ps, out_ap, pairs, reads):
        n = len(pairs)
        for i, (l, r) in enumerate(pairs):
            c.op("pe", (lambda l=l, r=r, i=i: nc.tensor.matmul(out_ap, lhsT=l, rhs=r, start=(i == 0), stop=(i == n - 1))),
                 reads=reads, writes=[ps], inc=(i == n - 1))

    def seg_of_tt(tt):
        return (tt * 128) // TSEG

    def precast_units(stg, stb):
        it = 0
        for l in range(2):
            for e in range(NEXP):
                for fq in range(NFQ):
                    idx = (l * NEXP + e) * NFQ + fq
                    for which in range(3):
                        sf = stg.next(); sb_ = stb.next()
                        if which == 0:
                            srcap = EG.t[l, e][:, fq * 512:(fq + 1) * 512].rearrange("(k p) n -> p k n", p=128); srct = EG; dstt = EGb
                        elif which == 1:
                            srcap = EU.t[l, e][:, fq * 512:(fq + 1) * 512].rearrange("(k p) n -> p k n", p=128); srct = EU; dstt = EUb
                        else:
                            srcap = ED.t[l, e][fq * 512:(fq + 1) * 512, :].rearrange("(k p) n -> p k n", p=128); srct = ED; dstt = EDb
                        if which < 2:
                            c.dma("sp", sf[:].rearrange("p (k n) -> p k n", k=KC), srcap, reads=[srct], writes=[sf])
                        else:
                            c.dma("sp", sf[:].rearrange("p (k n) -> p k n", k=4), srcap, reads=[srct], writes=[sf])
                        if it % 2 == 0:
                            act(lambda: nc.scalar.copy(out=sb_[:], in_=sf[:]), [sf], [sb_])
                        else:
                            dve(lambda: nc.vector.tensor_copy(out=sb_[:], in_=sf[:]), [sf], [sb_])
                        it += 1
                        c.dma("act", dstt.t[idx], sb_[:], reads=[sb_], writes=[dstt])
                        yield

    PRE = {"gen": None}

    c.push()
    cT = c.sbuf("cT", [128, KC, NSEG], F32)
    cTb = c.sbuf("cTb", [128, KC, NSEG], BF16)
    c.dma("sp", cT[:], c4T.t, writes=[cT])
    act(lambda: nc.scalar.activation(out=cTb[:], in_=cT[:], func=AF.Silu), [cT], [cTb])
    wmp = Pool(c, "wmod", [128, KC, 512], BF16, 2)
    modsb = c.sbuf("modsb", [NSEG, 6 * D], F32)
    bmodb = c.sbuf("bmodb", [NSEG, 6 * D], F32)
    for l in range(2):
        c.dma("sp", bmodb[:], b_mod.t[l:l + 1, :].partition_broadcast(NSEG) if False else b_mod.t[l:l + 1, :].to_broadcast([NSEG, 6 * D]),
              reads=[b_mod], writes=[bmodb])
        for nb in range(12):
            wt = wmp.next()
            c.dma("pool", wt[:], w_mod.t[l, :, nb * 512:(nb + 1) * 512].rearrange("(k p) n -> p k n", p=128),
                  reads=[w_mod], writes=[wt])
            ps = PSF.next()
            mm_group(ps, ps[0:NSEG, :], [(cTb[:, kc, :], wt[:, kc, :]) for kc in range(KC)], [cTb, wt])
            dve(lambda: nc.vector.tensor_tensor(out=modsb[:, nb * 512:(nb + 1) * 512], in0=ps[0:NSEG, :],
                                                in1=bmodb[:, nb * 512:(nb + 1) * 512], op=ALU.add), [ps, bmodb], [modsb])
        c.dma("act", MOD.t[l], modsb[:], reads=[modsb], writes=[MOD])
    c.pop()
    modT = c.sbuf("modT", [128, 2 * 6 * KC * NSEG], F32)

    def modT_ap(l, k, kc, seg):
        o = ((l * 6 + k) * NSEG + seg) * KC + kc
        return modT[:, o:o + 1]
    with nc.allow_non_contiguous_dma(reason="tiny modulation transpose load"):
        for l in range(2):
            for k in range(6):
                for seg in range(NSEG):
                    o = ((l * 6 + k) * NSEG + seg) * KC
                    c.dma("sp", modT[:, o:o + KC], MOD.t[l, seg, k * D:(k + 1) * D].rearrange("(k p) -> p k", p=128),
                          reads=[MOD], writes=[modT])
    dbg_dump("MOD", MOD, [2, NSEG, 6 * D])

    def phase_norm(l, which, src):
        c.push()
        nw = norm1_w if which == 0 else norm2_w
        ksh, ksc = (0, 1) if which == 0 else (3, 4)
        nwT = c.sbuf("nwT", [128, KC], F32)
        with nc.allow_non_contiguous_dma(reason="tiny"):
            c.dma("sp", nwT[:], nw.t[l, :].rearrange("(k p) -> p k", p=128), reads=[nw], writes=[nwT])
        Asc = c.sbuf("Asc", [128, KC * NSEG], F32)
        for seg in range(NSEG):
            o = ((l * 6 + ksc) * NSEG + seg) * KC
            dve(lambda: nc.vector.scalar_tensor_tensor(out=Asc[:, seg * KC:(seg + 1) * KC], in0=modT[:, o:o + KC], scalar=1.0,
                                                       in1=nwT[:], op0=ALU.add, op1=ALU.mult), [modT, nwT], [Asc])
        xp = Pool(c, "xa", [128, D], F32, 3)
        junk = c.sbuf("junk", [128, D], BF16)
        xnp = Pool(c, "xn", [128, D], BF16, 2)
        ssp = Pool(c, "ss", [128, 2], F32, 4)
        blkp = Pool(c, "xmblk", [128, KC, 512], BF16, 2)
        blk = None
        for tt in range(NTT):
            seg = seg_of_tt(tt)
            if tt % 4 == 0:
                blk = blkp.next()
            xt = xp.next()
            c.dma("sp", xt[:], src.t[tt * 128:(tt + 1) * 128, :], reads=[src], writes=[xt])
            ss = ssp.next()
            act(lambda: nc.scalar.activation(out=junk[:], in_=xt[:], func=AF.Square, accum_out=ss[:, 0:1]), [xt], [junk, ss])
            act(lambda: nc.scalar.activation(out=ss[:, 1:2], in_=ss[:, 0:1], func=AF.Sqrt, bias=EPS, scale=1.0 / D), [ss], [ss])
            dve(lambda: nc.vector.reciprocal(out=ss[:, 1:2], in_=ss[:, 1:2]), [ss], [ss])
            xn = xnp.next()
            dve(lambda: nc.vector.tensor_scalar(out=xn[:], in0=xt[:], scalar1=ss[:, 1:2], scalar2=None, op0=ALU.mult), [xt, ss], [xn])
            pb = PSB.next()
            for kc in range(KC):
                c.op("pe", lambda kc=kc: nc.tensor.transpose(pb[:, kc * 128:(kc + 1) * 128], xn[:, kc * 128:(kc + 1) * 128], ident_b[:]),
                     reads=[xn, ident_b], writes=[pb], inc=(kc == KC - 1))
            for kc in range(KC):
                a_ap = Asc[:, seg * KC + kc:seg * KC + kc + 1]
                b_ap = modT_ap(l, ksh, kc, seg)
                o_ap = blk[:, kc, (tt % 4) * 128:(tt % 4 + 1) * 128]
                i_ap = pb[:, kc * 128:(kc + 1) * 128]
                if kc % 2 == 0:
                    dve(lambda: nc.vector.tensor_scalar(out=o_ap, in0=i_ap, scalar1=a_ap, scalar2=b_ap, op0=ALU.mult, op1=ALU.add),
                        [pb, Asc, modT], [blk])
                else:
                    act(lambda: nc.scalar.activation(out=o_ap, in_=i_ap, func=AF.Identity, bias=b_ap, scale=a_ap), [pb, Asc, modT], [blk])
            if tt % 4 == 3:
                b0 = (tt // 4) * 512
                c.dma("act", XMT.t[:, :, b0:b0 + 512].rearrange("k p t -> p k t"), blk[:], reads=[blk], writes=[XMT])
        c.pop()

    def load_w(pool_t, l, col0, ncols):
        c.dma("pool", pool_t[:, :, 0:ncols], w_in.t[l, :, col0:col0 + ncols].rearrange("(k p) n -> p k n", p=128),
              reads=[w_in], writes=[pool_t])

    def load_bias_fm(t, l, col0, nft):
        with nc.allow_non_contiguous_dma(reason="tiny bias"):
            c.dma("sp", t[:, 0:nft], b_in.t[l, col0:col0 + nft * 128].rearrange("(k p) -> p k", p=128), reads=[b_in], writes=[t])

    def load_bias_row(t, l, col0, ncols):
        c.dma("sp", t[:, 0:ncols], b_in.t[l:l + 1, col0:col0 + ncols].to_broadcast([128, ncols]), reads=[b_in], writes=[t])

    def load_xmblk(t, blk):
        c.dma("sp", t[:], XMT.t[:, :, blk * 512:(blk + 1) * 512].rearrange("k p t -> p k t"), reads=[XMT], writes=[t])

    def phase_mlstm_proj(l, GA):
        c.push()
        Wqk = c.sbuf("Wqk", [128, KC, 2048], BF16)
        Wkvo = c.sbuf("Wkvo", [128, KC, 3072], BF16)
        Wg = c.sbuf("Wg", [128, KC, 16], BF16)
        load_w(Wqk, l, OFF["aq"], 2048)
        load_w(Wkvo, l, OFF["ak"], 3072)
        load_w(Wg, l, OFF["ag"], 16)
        bqk = c.sbuf("bqk", [128, 16], F32)
        load_bias_fm(bqk, l, OFF["aq"], 16)
        brow = c.sbuf("brow", [128, 3072 + 16], F32)
        load_bias_row(brow, l, OFF["ak"], 3072 + 16)
        xbp = Pool(c, "xb", [128, KC, 512], BF16, 2)
        stq = Pool(c, "stq", [128, 16, 512], BF16, 2)
        stk = Pool(c, "stk", [128, 3072], BF16, 2)
        tmpo = Pool(c, "tmpo", [128, 512], F32, 2)
        for blk in range(NBLK):
            xb = xbp.next()
            load_xmblk(xb, blk)
            sq = stq.next()
            for ft in range(16):
                ps = PSF.next()
                mm_group(ps, ps[:], [(Wqk[:, kc, ft * 128:(ft + 1) * 128], xb[:, kc, :]) for kc in range(KC)], [Wqk, xb])
                if ft % 2 == 0:
                    act(lambda: nc.scalar.activation(out=sq[:, ft, :], in_=ps[:], func=AF.Identity, bias=bqk[:, ft:ft + 1], scale=1.0),
                        [ps, bqk], [sq])
                else:
                    dve(lambda: nc.vector.tensor_scalar(out=sq[:, ft, :], in0=ps[:], scalar1=bqk[:, ft:ft + 1], scalar2=None, op0=ALU.add),
                        [ps, bqk], [sq])
            c.dma("act", QKT.t[:, :, blk * 512:(blk + 1) * 512].rearrange("k p t -> p k t"), sq[:], reads=[sq], writes=[QKT])
            for t4 in range(4):
                tt = blk * 4 + t4
                sk = stk.next()
                for nh in range(6):
                    ps = PSF.next()
                    mm_group(ps, ps[:], [(xb[:, kc, t4 * 128:(t4 + 1) * 128], Wkvo[:, kc, nh * 512:(nh + 1) * 512]) for kc in range(KC)], [Wkvo, xb])
                    if nh < 4:
                        dve(lambda: nc.vector.tensor_tensor(out=sk[:, nh * 512:(nh + 1) * 512], in0=ps[:], in1=brow[:, nh * 512:(nh + 1) * 512], op=ALU.add),
                            [ps, brow], [sk])
                    else:
                        tp = tmpo.next()
                        dve(lambda: nc.vector.tensor_tensor(out=tp[:], in0=ps[:], in1=brow[:, nh * 512:(nh + 1) * 512], op=ALU.add), [ps, brow], [tp])
                        act(lambda: nc.scalar.activation(out=sk[:, nh * 512:(nh + 1) * 512], in_=tp[:], func=AF.Sigmoid), [tp], [sk])
                c.dma("act", KVO.t[tt * 128:(tt + 1) * 128, :], sk[:], reads=[sk], writes=[KVO])
                ps = PSF.next()
                mm_group(ps, ps[:, 0:16], [(xb[:, kc, t4 * 128:(t4 + 1) * 128], Wg[:, kc, :]) for kc in range(KC)], [Wg, xb])
                dve(lambda: nc.vector.tensor_tensor(out=GA[:, tt, :], in0=ps[:, 0:16], in1=brow[:, 3072:3088], op=ALU.add), [ps, brow], [GA])
        c.pop()

    def phase_mlstm_scan(l, GA):
        c.push()
        LF = c.sbuf("LF", [128, NTT, 8], F32)
        IG = c.sbuf("IG", [128, NTT, 8], F32)
        BC = c.sbuf("BC", [128, NTT, 8], F32)
        E1 = c.sbuf("E1", [128, NTT, 8], F32)
        for d in range(2):
            dve(lambda: nc.vector.tensor_copy(out=IG[:, :, d * 4:(d + 1) * 4], in_=GA[:, :, d * 8:d * 8 + 4]), [GA], [IG])
            act(lambda: nc.scalar.activation(out=LF[:, :, d * 4:(d + 1) * 4], in_=GA[:, :, d * 8 + 4:d * 8 + 8], func=AF.Exp, scale=-1.0), [GA], [LF])
        act(lambda: nc.scalar.activation(out=LF[:], in_=LF[:], func=AF.Ln, bias=1.0, scale=1.0), [LF], [LF])
        dve(lambda: nc.vector.tensor_scalar(out=LF[:], in0=LF[:], scalar1=-1.0, scalar2=None, op0=ALU.mult), [LF], [LF])
        CW = 512 // 8
        for d in range(2):
            tri = triu_f if d == 0 else tril_f
            for c0 in range(0, NTT, CW):
                c1 = min(NTT, c0 + CW)
                n = (c1 - c0) * 4
                ps = PSF.next()
                for cc in range(c0, c1):
                    c.op("pe", lambda cc=cc: nc.tensor.matmul(ps[:, (cc - c0) * 4:(cc - c0 + 1) * 4], lhsT=tri[:], rhs=LF[:, cc, d * 4:(d + 1) * 4], start=True, stop=True),
                         reads=[tri, LF], writes=[ps], inc=(cc == c1 - 1))
                dve(lambda: nc.vector.tensor_copy(out=BC[:, c0:c1, d * 4:(d + 1) * 4], in_=ps[:, 0:n].rearrange("p (c h) -> p c h", h=4)), [ps], [BC])
        dve(lambda: nc.vector.tensor_tensor(out=E1[:], in0=IG[:], in1=BC[:], op=ALU.subtract), [IG, BC], [E1])
        act(lambda: nc.scalar.activation(out=E1[:], in_=E1[:], func=AF.Exp), [E1], [E1])
        dve(lambda: nc.vector.tensor_scalar(out=E1[:], in0=E1[:], scalar1=1.0 / 16.0, scalar2=None, op0=ALU.mult), [E1], [E1])

        nwb = c.sbuf("nwb", [128, D], F32)
        c.dma("sp", nwb[:], mlstm_norm_w.t[l:l + 1, :].to_broadcast([128, D]), reads=[mlstm_norm_w], writes=[nwb])
        qtp = Pool(c, "qT", [128, 8, 128], BF16, 2)
        ktp = Pool(c, "kT", [128, 8, 128], BF16, 2)
        kvp = Pool(c, "kv", [128, 3072], BF16, 2)
        vxp = Pool(c, "vx", [128, 4, 257], BF16, 2)
        for v in vxp.tiles:
            pool_op(lambda v=v: nc.gpsimd.memset(v[:], 1.0), writes=[v])
        dgp = Pool(c, "dg", [128, 4, 128], F32, 2)
        e2p = Pool(c, "e2", [128, 4, 128], F32, 2)
        qpp = Pool(c, "qp", [128, 8, 128], BF16, 2)
        stp = Pool(c, "sT", [128, 128], BF16, 4)
        kpp = Pool(c, "kp", [128, 256], BF16, 4)
        hop = Pool(c, "ho", [128, D], F32, 2)
        smp = Pool(c, "sm", [128, 4], F32, 8)
        Cf = [c.sbuf("Cf%d" % h, [128, 2, 257], F32) for h in range(4)]
        Cb = [c.sbuf("Cb%d" % h, [128, 2, 257], BF16) for h in range(4)]
        hfp = Pool(c, "hfl", [128, D], F32, 2)
        hbp = Pool(c, "hab", [128, D], BF16, 2)
        stt = Pool(c, "bst", [128, 4, 8], F32, 2)
        hts = Pool(c, "hts", [128, KC, 128], BF16, 2)

        pre_n = 0
        if with_moe and l == 0:
            pstg = Pool(c, "pcs", [128, 4096], F32, 3)
            pstb = Pool(c, "pcb", [128, 4096], BF16, 3)
            PRE["gen"] = precast_units(pstg, pstb)
            total_units = 2 * NEXP * NFQ * 3
            pre_n = -(-total_units // (2 * NTT))
        for d in range(2):
            for h in range(4):
                dve(lambda h=h: nc.vector.memset(Cf[h][:], 0.0), writes=[Cf[h]])
                dve(lambda h=h: nc.vector.memset(Cb[h][:], 0.0), writes=[Cb[h]])
            order = range(NTT) if d == 0 else range(NTT - 1, -1, -1)
            msk = mask_f if d == 0 else mask_b
            for ch in order:
                t0 = ch * 128
                first_of_seg = (ch % CPS == 0) if d == 0 else (ch % CPS == CPS - 1)
                is_first = (ch == 0) if d == 0 else (ch == NTT - 1)
                if first_of_seg and not is_first:
                    for h in range(4):
                        dve(lambda h=h: nc.vector.tensor_scalar(out=Cf[h][:], in0=Cf[h][:], scalar1=keep[:, 0:1], scalar2=None, op0=ALU.mult), [Cf[h], keep], [Cf[h]])
                        act(lambda h=h: nc.scalar.copy(out=Cb[h][:], in_=Cf[h][:]), [Cf[h]], [Cb[h]])
                for _ in range(pre_n):
                    if PRE["gen"] is not None and next(PRE["gen"], "done") == "done":
                        PRE["gen"] = None
                qT = qtp.next(); kT = ktp.next(); kv = kvp.next(); vx = vxp.next()
                c.dma("sp", qT[:], QKT.t[0:8, :, t0:t0 + 128].rearrange("k p t -> p k t"), reads=[QKT], writes=[qT])
                c.dma("sp", kT[:], QKT.t[8:16, :, t0:t0 + 128].rearrange("k p t -> p k t"), reads=[QKT], writes=[kT])
                c.dma("sp", kv[:], KVO.t[t0:t0 + 128, :], reads=[KVO], writes=[kv])
                dve(lambda: nc.vector.tensor_copy(out=vx[:, :, 0:256], in_=kv[:, 1024:2048].rearrange("p (h e) -> p h e", h=4)), [kv], [vx])
                dg = dgp.next()
                dve(lambda: nc.vector.tensor_tensor(out=dg[:], in0=ident_f[:].unsqueeze(1).to_broadcast([128, 4, 128]),
                                                    in1=BC[:, ch, d * 4:d * 4 + 4].unsqueeze(2).to_broadcast([128, 4, 128]), op=ALU.mult), [ident_f, BC], [dg])
                ps = PSF.next()
                c.op("pe", lambda: nc.tensor.matmul(ps[:], lhsT=ones_f[:], rhs=dg[:].rearrange("p h i -> p (h i)"), start=True, stop=True), [ones_f, dg], [ps])
                e2 = e2p.next()
                act(lambda: nc.scalar.activation(out=e2[:].rearrange("p h i -> p (h i)"), in_=ps[:], func=AF.Exp), [ps], [e2])
                qp = qpp.next()
                for dc in range(2):
                    dve(lambda dc=dc: nc.vector.tensor_tensor(out=qp[:].rearrange("p (h two) i -> p h two i", two=2)[:, :, dc, :],
                                                              in0=qT[:].rearrange("p (h two) i -> p h two i", two=2)[:, :, dc, :], in1=e2[:], op=ALU.mult), [qT, e2], [qp])
                ho = hop.next()
                elast = 127 if d == 0 else 0
                for h in range(4):
                    col = d * 4 + h
                    psA = PSF.next()
                    mm_group(psA, psA[:, 0:128], [(kT[:, 2 * h + dc, :], qp[:, 2 * h + dc, :]) for dc in range(2)], [kT, qp])
                    sT = stp.next()
                    dve(lambda: nc.vector.scalar_tensor_tensor(out=sT[:], in0=psA[:, 0:128], scalar=E1[:, ch, col:col + 1], in1=msk[:], op0=ALU.mult, op1=ALU.mult),
                        [psA, E1, msk], [sT])
                    kp = kpp.next()
                    act(lambda: nc.scalar.activation(out=kp[:], in_=kv[:, h * 256:(h + 1) * 256], func=AF.Copy, scale=E1[:, ch, col:col + 1]), [kv, E1], [kp])
                    psN = PSF.next()
                    mm_group(psN, psN[:, 0:257], [(sT[:], vx[:, h, :])] + [(qp[:, 2 * h + dc, :], Cb[h][:, dc, :]) for dc in range(2)], [sT, vx, qp, Cb[h]])
                    sm = smp.next()
                    act(lambda: nc.scalar.activation(out=sm[:, 0:1], in_=psN[:, 256:257], func=AF.Abs), [psN], [sm])
                    dve(lambda: nc.vector.tensor_scalar(out=sm[:, 0:1], in0=sm[:, 0:1], scalar1=1.0, scalar2=None, op0=ALU.max), [sm], [sm])
                    dve(lambda: nc.vector.reciprocal(out=sm[:, 1:2], in_=sm[:, 0:1]), [sm], [sm])
                    act(lambda: nc.scalar.activation(out=ho[:, h * 256:(h + 1) * 256], in_=psN[:, 0:256], func=AF.Copy, scale=sm[:, 1:2]), [psN, sm], [ho])
                    for dc in range(2):
                        psC = PSF.next()
                        mm_group(psC, psC[:, 0:257], [(kp[:, dc * 128:(dc + 1) * 128], vx[:, h, :])], [kp, vx])
                        dve(lambda: nc.vector.tensor_tensor(out=Cf[h][:, dc, :], in0=psC[:, 0:257], in1=Cf[h][:, dc, :], op=ALU.add), [psC, Cf[h]], [Cf[h]])
                    dve(lambda: nc.vector.tensor_scalar(out=Cf[h][:], in0=Cf[h][:], scalar1=e2[:, h, elast:elast + 1], scalar2=None, op0=ALU.mult), [Cf[h], e2], [Cf[h]])
                    act(lambda: nc.scalar.copy(out=Cb[h][:], in_=Cf[h][:]), [Cf[h]], [Cb[h]])
                if d == 0:
                    c.dma("act", HF.t[t0:t0 + 128, :], ho[:], reads=[ho], writes=[HF])
                else:
                    hf = hfp.next()
                    c.dma("sp", hf[:], HF.t[t0:t0 + 128, :], reads=[HF], writes=[hf])
                    dve(lambda: nc.vector.tensor_tensor(out=ho[:], in0=ho[:], in1=hf[:], op=ALU.add), [ho, hf], [ho])
                    st = stt.next()
                    for h in range(4):
                        dve(lambda h=h: nc.vector.bn_stats(out=st[:, h, 0:6], in_=ho[:, h * 256:(h + 1) * 256]), [ho], [st])
                        dve(lambda h=h: nc.vector.bn_aggr(out=st[:, h, 6:8], in_=st[:, h, 0:6]), [st], [st])
                        act(lambda h=h: nc.scalar.activation(out=st[:, h, 7:8], in_=st[:, h, 7:8], func=AF.Sqrt, bias=EPS, scale=1.0), [st], [st])
                        dve(lambda h=h: nc.vector.reciprocal(out=st[:, h, 7:8], in_=st[:, h, 7:8]), [st], [st])
                        dve(lambda h=h: nc.vector.tensor_scalar(out=ho[:, h * 256:(h + 1) * 256], in0=ho[:, h * 256:(h + 1) * 256], scalar1=st[:, h, 6:7],
                                                                scalar2=st[:, h, 7:8], op0=ALU.subtract, op1=ALU.mult), [ho, st], [ho])
                    dve(lambda: nc.vector.tensor_tensor(out=ho[:], in0=ho[:], in1=nwb[:], op=ALU.mult), [ho, nwb], [ho])
                    hb = hbp.next()
                    dve(lambda: nc.vector.tensor_tensor(out=hb[:], in0=ho[:], in1=kv[:, 2048:3072], op=ALU.mult), [ho, kv], [hb])
                    pb = PSB.next()
                    for kc in range(KC):
                        c.op("pe", lambda kc=kc: nc.tensor.transpose(pb[:, kc * 128:(kc + 1) * 128], hb[:, kc * 128:(kc + 1) * 128], ident_b[:]),
                             reads=[hb, ident_b], writes=[pb], inc=(kc == KC - 1))
                    ht = hts.next()
                    act(lambda: nc.scalar.copy(out=ht[:].rearrange("p k t -> p (k t)"), in_=pb[:]), [pb], [ht])
                    c.dma("act", HT[0].t[:, :, t0:t0 + 128].rearrange("k p t -> p k t"), ht[:], reads=[ht], writes=[HT[0]])
        while PRE["gen"] is not None:
            if next(PRE["gen"], "done") == "done":
                PRE["gen"] = None
        c.pop()


    def phase_na_proj(l):
        c.push()
        Wqk = c.sbuf("nWqk", [128, KC, 2048], BF16)
        Wv = c.sbuf("nWv", [128, KC, 1024], BF16)
        load_w(Wqk, l, OFF["bq"], 2048)
        load_w(Wv, l, OFF["bv"], 1024)
        bqk = c.sbuf("nbqk", [128, 16], F32)
        load_bias_fm(bqk, l, OFF["bq"], 16)
        brow = c.sbuf("nbrow", [128, 1024], F32)
        load_bias_row(brow, l, OFF["bv"], 1024)
        xbp = Pool(c, "nxb", [128, KC, 512], BF16, 2)
        stq = Pool(c, "nstq", [128, 16, 512], BF16, 2)
        stv = Pool(c, "nstv", [128, 1024], BF16, 2)
        for blk in range(NBLK):
            xb = xbp.next()
            load_xmblk(xb, blk)
            sq = stq.next()
            for ft in range(16):
                ps = PSF.next()
                mm_group(ps, ps[:], [(Wqk[:, kc, ft * 128:(ft + 1) * 128], xb[:, kc, :]) for kc in range(KC)], [Wqk, xb])
                sc = 0.125 if ft < 8 else 1.0
                dve(lambda: nc.vector.tensor_scalar(out=sq[:, ft, :], in0=ps[:], scalar1=bqk[:, ft:ft + 1], scalar2=sc, op0=ALU.add, op1=ALU.mult),
                    [ps, bqk], [sq])
            c.dma("act", QKT.t[:, :, blk * 512:(blk + 1) * 512].rearrange("k p t -> p k t"), sq[:], reads=[sq], writes=[QKT])
            for t4 in range(4):
                tt = blk * 4 + t4
                sv = stv.next()
                for nh in range(2):
                    ps = PSF.next()
                    mm_group(ps, ps[:], [(xb[:, kc, t4 * 128:(t4 + 1) * 128], Wv[:, kc, nh * 512:(nh + 1) * 512]) for kc in range(KC)], [Wv, xb])
                    dve(lambda: nc.vector.tensor_tensor(out=sv[:, nh * 512:(nh + 1) * 512], in0=ps[:], in1=brow[:, nh * 512:(nh + 1) * 512], op=ALU.add),
                        [ps, brow], [sv])
                c.dma("act", NV.t[tt * 128:(tt + 1) * 128, :], sv[:], reads=[sv], writes=[NV])
        c.pop()

    def phase_na_attn(l):
        c.push()
        T2 = c.sbuf("T2", [128, 16, 16, 64], BF16)
        tmpb = Pool(c, "btmp", [128, 4096], F32, 2)
        for part in range(4):
            tb = tmpb.next()
            c.dma("sp", tb[:], btab.t[l, :, part * 4096:(part + 1) * 4096], reads=[btab], writes=[tb])
            act(lambda: nc.scalar.activation(out=T2[:, part * 4:(part + 1) * 4, :, :].rearrange("p h x q -> p (h x q)"), in_=tb[:], func=AF.Exp), [tb], [T2])
        rvs = c.sbuf("rvs", [128, ROWS * 8], F32)
        c.dma("sp", rvs[:], rv_in.t, reads=[rv_in], writes=[rvs])
        NR = 10
        kring = [c.sbuf("kr%d" % i, [128, 8, 128], BF16) for i in range(NR)]
        vring = [c.sbuf("vr%d" % i, [128, 1024], BF16) for i in range(NR)]
        loaded = {}
        qrp = Pool(c, "qrow", [128, 8, 2, 64], BF16, 3)
        for q_ in qrp.tiles:
            dve(lambda q_=q_: nc.vector.memset(q_[:], 0.0), [], [q_])
        pep = Pool(c, "pexp", [128, 512], BF16, 3)
        ptp = Pool(c, "ptall", [128, 8, 8, 64], BF16, 2)
        recp = Pool(c, "nrec", [64, 512], F32, 2)
        outp = Pool(c, "nout", [64, 8, 64], BF16, 2)
        nch = ROWS // 2
        rvP = _rv_table(ROWS, True); rvS = _rv_table(ROWS, False)
        for R in range(ROWS):
            cs = min(max((R - 7) // 2, 0), nch - 8)
            valid = [s_ for s_ in range(8) if 0 <= 2 * (cs + s_) - R + 8 <= 15
                     and (rvP[:, R * 8 + s_].any() or rvS[:, R * 8 + s_].any())]
            s_lo, s_hi = valid[0], valid[-1]
            assert valid == list(range(s_lo, s_hi + 1))
            ns = len(valid)
            x_lo = 2 * (cs + s_lo) - R + 8
            need_rv = not (rvP[:, R * 8 + s_lo:R * 8 + s_hi + 1].all() and rvS[:, R * 8 + s_lo:R * 8 + s_hi + 1].all())
            for s_ in valid:
                ch = cs + s_
                if loaded.get(ch % NR) != ch:
                    c.dma("sp", kring[ch % NR][:], QKT.t[8:16, :, ch * 128:(ch + 1) * 128].rearrange("k p t -> p k t"), reads=[QKT], writes=[kring[ch % NR]])
                    c.dma("sp", vring[ch % NR][:], NV.t[ch * 128:(ch + 1) * 128, :], reads=[NV], writes=[vring[ch % NR]])
                    loaded[ch % NR] = ch
            qr = qrp.next()
            c.dma("sp", qr[0:64, :, 0, :], QKT.t[0:8, 0:64, R * 64:(R + 1) * 64].rearrange("k p t -> p k t"), reads=[QKT], writes=[qr])
            c.dma("sp", qr[64:128, :, 1, :], QKT.t[0:8, 64:128, R * 64:(R + 1) * 64].rearrange("k p t -> p k t"), reads=[QKT], writes=[qr])
            groups = [list(range(g0, min(g0 + 4, ns))) for g0 in range(0, ns, 4)]
            for hh in range(2):
                pt = ptp.next()
                psnum = PSL.next()
                for hpl in range(4):
                    ft = hh * 4 + hpl
                    for grp in groups:
                        g = len(grp)
                        psS = PSF.next()
                        for gi, si in enumerate(grp):
                            kt = kring[(cs + valid[si]) % NR]
                            c.op("pe", lambda: nc.tensor.matmul(psS[:, gi * 128:(gi + 1) * 128], lhsT=kt[:, ft, :], rhs=qr[:, ft, :, :].rearrange("p two q -> p (two q)"), start=True, stop=True),
                                 reads=[kt, qr], writes=[psS], inc=(gi == g - 1))
                        pe_t = pep.next()
                        act(lambda: nc.scalar.activation(out=pe_t[:, 0:g * 128], in_=psS[:, 0:g * 128], func=AF.Exp), [psS], [pe_t])
                        xs0 = x_lo + 2 * grp[0]
                        dve(lambda: nc.vector.tensor_tensor(out=pt[:, 2 * hpl:2 * hpl + 2, grp[0]:grp[0] + g, :].rearrange("p two s q -> p s two q"),
                                                            in0=pe_t[:, 0:g * 128].rearrange("p (s two q) -> p s two q", two=2, q=64),
                                                            in1=T2[:, 2 * ft:2 * ft + 2, xs0:xs0 + 2 * g - 1:2, :].rearrange("p two s q -> p s two q"), op=ALU.mult), [pe_t, T2], [pt])
                    if need_rv:
                        for two in range(2):
                            hl = 2 * hpl + two
                            dve(lambda: nc.vector.tensor_tensor(out=pt[:, hl, 0:ns, :], in0=pt[:, hl, 0:ns, :],
                                                                in1=rvs[:, R * 8 + s_lo:R * 8 + s_hi + 1].unsqueeze(2).to_broadcast([128, ns, 64]), op=ALU.mult), [pt, rvs], [pt])
                    for two in range(2):
                        hl = 2 * hpl + two
                        h = hh * 8 + hl
                        for si, s_ in enumerate(valid):
                            vt = vring[(cs + s_) % NR]
                            c.op("pe", lambda: nc.tensor.matmul(psnum[0:64, hl * 64:(hl + 1) * 64], lhsT=vt[:, h * 64:(h + 1) * 64], rhs=pt[:, hl, si, :], start=(si == 0), stop=(si == ns - 1)),
                                 reads=[vt, pt], writes=[psnum], inc=(si == ns - 1))
                psden = PSF.next()
                for si in range(ns):
                    c.op("pe", lambda: nc.tensor.matmul(psden[0:64, :], lhsT=ones_b[:, 0:64], rhs=pt[:, :, si, :], start=(si == 0), stop=(si == ns - 1)),
                         reads=[ones_b, pt], writes=[psden], inc=(si == ns - 1))
                rec = recp.next()
                dve(lambda: nc.vector.reciprocal(out=rec[:], in_=psden[0:64, :]), [psden], [rec])
                ot = outp.next()
                dve(lambda: nc.vector.tensor_tensor(out=ot[:].rearrange("p h q -> p (h q)"), in0=psnum[0:64, :], in1=rec[:], op=ALU.mult), [psnum, rec], [ot])
                c.dma("act", HT[1].t[hh * 4:(hh + 1) * 4, :, R * 64:(R + 1) * 64].rearrange("f (two d) t -> d (f two) t", two=2), ot[:], reads=[ot], writes=[HT[1]])
        c.pop()

    def phase_lru_proj(l):
        c.push()
        W = c.sbuf("lW", [128, KC, 2048], BF16)
        load_w(W, l, OFF["cx"], 2048)
        bfm = c.sbuf("lbfm", [128, 16], F32)
        load_bias_fm(bfm, l, OFF["cx"], 16)
        xbp = Pool(c, "lxb", [128, KC, 512], BF16, 2)
        stp_ = Pool(c, "lst", [128, 16, 512], F32, 2)
        for blk in range(NBLK):
            xb = xbp.next()
            load_xmblk(xb, blk)
            st = stp_.next()
            for ft in range(16):
                ps = PSF.next()
                mm_group(ps, ps[:], [(W[:, kc, ft * 128:(ft + 1) * 128], xb[:, kc, :]) for kc in range(KC)], [W, xb])
                if ft % 2 == 0:
                    act(lambda: nc.scalar.activation(out=st[:, ft, :], in_=ps[:], func=AF.Identity, bias=bfm[:, ft:ft + 1], scale=1.0), [ps, bfm], [st])
                else:
                    dve(lambda: nc.vector.tensor_scalar(out=st[:, ft, :], in0=ps[:], scalar1=bfm[:, ft:ft + 1], scalar2=None, op0=ALU.add), [ps, bfm], [st])
            c.dma("act", CXY.t[:, :, blk * 512:(blk + 1) * 512].rearrange("k p t -> p k t"), st[:], reads=[st], writes=[CXY])
        c.pop()

    def phase_lru_scan(l):
        c.push()
        cw = c.sbuf("cw", [128, 8, 4], F32)
        cbv = c.sbuf("cbv", [128, 8], F32)
        gb = c.sbuf("gb", [128, 4, 8], F32)
        cL = c.sbuf("cL", [128, 2, 8], F32)
        ncw = c.sbuf("ncw", [128, 8, 4], F32)
        with nc.allow_non_contiguous_dma(reason="tiny per-channel params"):
            for j in range(4):
                c.dma("sp", cw[:, :, j], conv_w.t[l, j, :].rearrange("(f p) -> p f", p=128), reads=[conv_w], writes=[cw])
            c.dma("sp", cbv[:], conv_b.t[l, :].rearrange("(f p) -> p f", p=128), reads=[conv_b], writes=[cbv])
            for d in range(2):
                c.dma("sp", gb[:, d * 2 + 0, :], lru_ba.t[l, d, :].rearrange("(f p) -> p f", p=128), reads=[lru_ba], writes=[gb])
                c.dma("sp", gb[:, d * 2 + 1, :], lru_bx.t[l, d, :].rearrange("(f p) -> p f", p=128), reads=[lru_bx], writes=[gb])
                c.dma("sp", cL[:, d, :], lru_L.t[l, d, :].rearrange("(f p) -> p f", p=128), reads=[lru_L], writes=[cL])
        act(lambda: nc.scalar.activation(out=cL[:], in_=cL[:], func=AF.Exp, scale=-1.0), [cL], [cL])
        act(lambda: nc.scalar.activation(out=cL[:], in_=cL[:], func=AF.Ln, bias=1.0, scale=1.0), [cL], [cL])
        dve(lambda: nc.vector.tensor_scalar(out=cL[:], in0=cL[:], scalar1=-8.0, scalar2=None, op0=ALU.mult), [cL], [cL])
        dve(lambda: nc.vector.tensor_scalar(out=ncw[:], in0=cw[:], scalar1=nkeep[:, 0:1], scalar2=-1.0, op0=ALU.mult, op1=ALU.mult), [cw, nkeep], [ncw])
        bd = c.sbuf("bd", [128, 4, 8, 128], BF16)
        for g in range(4):
            c.dma("pool", bd[:, g, :, :], lru_bd.t[l, g].rearrange("f p j -> p f j"), reads=[lru_bd], writes=[bd])
        cxp = Pool(c, "cxhf", [128, Tn], F32, 1)
        xcp = Pool(c, "xc", [128, Tn], F32, 1)
        xbp = Pool(c, "xcb", [128, Tn], BF16, 1)
        Ap = Pool(c, "lA", [128, TSEG], F32, 2)
        Bp = Pool(c, "lB", [128, TSEG], F32, 2)
        hbp = Pool(c, "lhb", [128, TSEG], F32, 1)
        cyp = Pool(c, "lcy", [128, TSEG], F32, 2)
        t5 = Pool(c, "lt", [128, 512], F32, 6)
        car = Pool(c, "lcar", [128, 1], F32, 4)
        obp = Pool(c, "lob", [128, TSEG], BF16, 2)
        g1p = Pool(c, "lg1", [128, TSEG], F32, 1)
        for ft in range(8):
            cx = cxp.next(); xc = xcp.next(); xcb = xbp.next()
            c.dma("sp", cx[:], CXY.t[ft], reads=[CXY], writes=[cx])
            w = lambda j: cw[:, ft, j:j + 1]
            dve(lambda: nc.vector.tensor_scalar(out=xc[:], in0=cx[:], scalar1=w(2), scalar2=cbv[:, ft:ft + 1], op0=ALU.mult, op1=ALU.add), [cx, cw, cbv], [xc])
            dve(lambda: nc.vector.scalar_tensor_tensor(out=xc[:, 1:Tn], in0=cx[:, 0:Tn - 1], scalar=w(1), in1=xc[:, 1:Tn], op0=ALU.mult, op1=ALU.add), [cx, cw, xc], [xc])
            dve(lambda: nc.vector.scalar_tensor_tensor(out=xc[:, 2:Tn], in0=cx[:, 0:Tn - 2], scalar=w(0), in1=xc[:, 2:Tn], op0=ALU.mult, op1=ALU.add), [cx, cw, xc], [xc])
            dve(lambda: nc.vector.scalar_tensor_tensor(out=xc[:, 0:Tn - 1], in0=cx[:, 1:Tn], scalar=w(3), in1=xc[:, 0:Tn - 1], op0=ALU.mult, op1=ALU.add), [cx, cw, xc], [xc])
            nw_ = lambda j: ncw[:, ft, j:j + 1]
            for sgi in range(1, NSEG):
                b0 = sgi * TSEG
                for (dst_, src_, j) in ((b0, b0 - 1, 1), (b0, b0 - 2, 0), (b0 + 1, b0 - 1, 0), (b0 - 1, b0, 3)):
                    dve(lambda: nc.vector.scalar_tensor_tensor(out=xc[:, dst_:dst_ + 1], in0=cx[:, src_:src_ + 1], scalar=nw_(j), in1=xc[:, dst_:dst_ + 1],
                                                               op0=ALU.mult, op1=ALU.add), [cx, ncw, xc], [xc])
            act(lambda: nc.scalar.copy(out=xcb[:], in_=xc[:]), [xc], [xcb])
            hf = cx
            prev = None
            for d in range(2):
                segs = range(NSEG) if d == 0 else range(NSEG - 1, -1, -1)
                prev = None
                for sg in segs:
                    A = Ap.next(); Bt = Bp.next()
                    for b4 in range(TSEG // 512):
                        t0 = sg * TSEG + b4 * 512
                        lo_, hi_ = b4 * 512, (b4 + 1) * 512
                        psr = PSF.next()
                        mm_group(psr, psr[:], [(bd[:, d * 2 + 0, ft, :], xcb[:, t0:t0 + 512])], [bd, xcb])
                        psi = PSF.next()
                        mm_group(psi, psi[:], [(bd[:, d * 2 + 1, ft, :], xcb[:, t0:t0 + 512])], [bd, xcb])
                        r_ = t5.next(); i_ = t5.next(); u_ = t5.next()
                        act(lambda: nc.scalar.activation(out=r_[:], in_=psr[:], func=AF.Sigmoid, bias=gb[:, d * 2 + 0, ft:ft + 1], scale=1.0), [psr, gb], [r_])
                        act(lambda: nc.scalar.activation(out=i_[:], in_=psi[:], func=AF.Sigmoid, bias=gb[:, d * 2 + 1, ft:ft + 1], scale=1.0), [psi, gb], [i_])
                        act(lambda: nc.scalar.activation(out=A[:, lo_:hi_], in_=r_[:], func=AF.Exp, scale=cL[:, d, ft:ft + 1]), [r_, cL], [A])
                        dve(lambda: nc.vector.tensor_tensor(out=u_[:], in0=A[:, lo_:hi_], in1=A[:, lo_:hi_], op=ALU.mult), [A], [u_])
                        dve(lambda: nc.vector.tensor_scalar(out=u_[:], in0=u_[:], scalar1=-1.0, scalar2=1.0, op0=ALU.mult, op1=ALU.add), [u_], [u_])
                        act(lambda: nc.scalar.activation(out=u_[:], in_=u_[:], func=AF.Sqrt), [u_], [u_])
                        dve(lambda: nc.vector.tensor_tensor(out=i_[:], in0=i_[:], in1=xc[:, t0:t0 + 512], op=ALU.mult), [i_, xc], [i_])
                        dve(lambda: nc.vector.tensor_tensor(out=Bt[:, lo_:hi_], in0=i_[:], in1=u_[:], op=ALU.mult), [i_, u_], [Bt])
                    s0, s1 = sg * TSEG, (sg + 1) * TSEG
                    if prev is None:
                        init = 0.0
                        rd_init = []
                    else:
                        cr = car.next()
                        dve(lambda: nc.vector.tensor_scalar(out=cr[:], in0=prev[0], scalar1=keep[:, 0:1], scalar2=None, op0=ALU.mult), [prev[1], keep], [cr])
                        init = cr[:, 0:1]
                        rd_init = [cr]
                    if d == 0:
                        dve(lambda: nc.vector.tensor_tensor_scan(out=hf[:, s0:s1], data0=A[:], data1=Bt[:], initial=init, op0=ALU.mult, op1=ALU.add),
                            [A, Bt] + rd_init, [hf])
                        prev = (hf[:, s1 - 1:s1], hf)
                    else:
                        hb = hbp.next()
                        dve(lambda: nc.vector.tensor_tensor_scan(out=hb[:, ::-1], data0=A[:, ::-1], data1=Bt[:, ::-1], initial=init, op0=ALU.mult, op1=ALU.add),
                            [A, Bt] + rd_init, [hb])
                        cr2 = car.next()
                        dve(lambda: nc.vector.tensor_copy(out=cr2[:], in_=hb[:, 0:1]), [hb], [cr2])
                        prev = (cr2[:, 0:1], cr2)
                        dve(lambda: nc.vector.tensor_tensor(out=hb[:], in0=hb[:], in1=hf[:, s0:s1], op=ALU.add), [hb, hf], [hb])
                        cy = cyp.next()
                        c.dma("sp", cy[:], CXY.t[8 + ft, :, s0:s1], reads=[CXY], writes=[cy])
                        g1 = g1p.next()
                        act(lambda: nc.scalar.activation(out=g1[:], in_=cy[:], func=AF.Square), [cy], [g1])
                        dve(lambda: nc.vector.tensor_scalar(out=g1[:], in0=g1[:], scalar1=0.044715, scalar2=1.0, op0=ALU.mult, op1=ALU.add), [g1], [g1])
                        dve(lambda: nc.vector.tensor_tensor(out=g1[:], in0=g1[:], in1=cy[:], op=ALU.mult), [g1, cy], [g1])
                        act(lambda: nc.scalar.activation(out=g1[:], in_=g1[:], func=AF.Sigmoid, scale=1.5957691216057308), [g1], [g1])
                        dve(lambda: nc.vector.tensor_tensor(out=g1[:], in0=g1[:], in1=cy[:], op=ALU.mult), [g1, cy], [g1])
                        ob = obp.next()
                        dve(lambda: nc.vector.tensor_tensor(out=ob[:], in0=g1[:], in1=hb[:], op=ALU.mult), [g1, hb], [ob])
                        c.dma("act", HT[2].t[ft, :, s0:s1], ob[:], reads=[ob], writes=[HT[2]])
        c.pop()

    def phase_merge(l, src, dst):
        c.push()
        Wmg = c.sbuf("Wmg", [128, KC, 3072], BF16)
        load_w(Wmg, l, OFF["mg"], 3072)
        Wb = [c.sbuf("Wbr%d" % i, [128, KC, D], BF16) for i in range(3)]
        for i in range(3):
            c.dma("pool", Wb[i][:], w_br[i].t[l].rearrange("(k p) n -> p k n", p=128), reads=[w_br[i]], writes=[Wb[i]])
        Wo = c.sbuf("Wo", [128, KC, D], BF16)
        c.dma("pool", Wo[:], w_out.t[l].rearrange("(k p) n -> p k n", p=128), reads=[w_out], writes=[Wo])
        bmg = c.sbuf("bmg", [128, 24], F32)
        load_bias_fm(bmg, l, OFF["mg"], 24)
        g1b = c.sbuf("g1b", [128, D], F32)
        xb = c.sbuf("fxb", [128, KC, 512], BF16)
        hT = [c.sbuf("fhT%d" % i, [128, KC, 512], BF16) for i in range(3)]
        mt = c.sbuf("mt", [128, KC, 512], F32)
        mtb = c.sbuf("mtb", [128, KC, 512], BF16)
        sgp = Pool(c, "fsg", [128, 512], F32, 2)
        tmp = Pool(c, "ftmp", [128, 512], F32, 2)
        xtp = Pool(c, "fx", [128, D], F32, 2)
        cur_seg = -1
        for blk in range(NBLK):
            seg = (blk * 512) // TSEG
            if seg != cur_seg:
                c.dma("sp", g1b[:], MOD.t[l, seg:seg + 1, 2 * D:3 * D].to_broadcast([128, D]), reads=[MOD], writes=[g1b])
                cur_seg = seg
            load_xmblk(xb, blk)
            for i in range(3):
                c.dma("sp", hT[i][:], HT[i].t[:, :, blk * 512:(blk + 1) * 512].rearrange("k p t -> p k t"), reads=[HT[i]], writes=[hT[i]])
            for i in range(3):
                for ft in range(8):
                    psg = PSF.next()
                    col = i * 1024 + ft * 128
                    mm_group(psg, psg[:], [(Wmg[:, kc, col:col + 128], xb[:, kc, :]) for kc in range(KC)], [Wmg, xb])
                    sg_ = sgp.next()
                    act(lambda: nc.scalar.activation(out=sg_[:], in_=psg[:], func=AF.Sigmoid, bias=bmg[:, i * 8 + ft:i * 8 + ft + 1], scale=1.0), [psg, bmg], [sg_])
                    psb_ = PSF.next()
                    mm_group(psb_, psb_[:], [(Wb[i][:, kc, ft * 128:(ft + 1) * 128], hT[i][:, kc, :]) for kc in range(KC)], [Wb[i], hT[i]])
                    if i == 0:
                        dve(lambda: nc.vector.tensor_tensor(out=mt[:, ft, :], in0=psb_[:], in1=sg_[:], op=ALU.mult), [psb_, sg_], [mt])
                    else:
                        tm = tmp.next()
                        dve(lambda: nc.vector.tensor_tensor(out=tm[:], in0=psb_[:], in1=sg_[:], op=ALU.mult), [psb_, sg_], [tm])
                        if i == 1:
                            dve(lambda: nc.vector.tensor_tensor(out=mt[:, ft, :], in0=mt[:, ft, :], in1=tm[:], op=ALU.add), [mt, tm], [mt])
                        else:
                            dve(lambda: nc.vector.tensor_tensor(out=mtb[:, ft, :], in0=mt[:, ft, :], in1=tm[:], op=ALU.add), [mt, tm], [mtb])
            for t4 in range(4):
                tt = blk * 4 + t4
                xt = xtp.next()
                c.dma("sp", xt[:], src.t[tt * 128:(tt + 1) * 128, :], reads=[src], writes=[xt])
                for nh in range(2):
                    ps = PSF.next()
                    mm_group(ps, ps[:], [(mtb[:, kc, t4 * 128:(t4 + 1) * 128], Wo[:, kc, nh * 512:(nh + 1) * 512]) for kc in range(KC)], [mtb, Wo])
                    tm = tmp.next()
                    dve(lambda: nc.vector.tensor_tensor(out=tm[:], in0=ps[:], in1=g1b[:, nh * 512:(nh + 1) * 512], op=ALU.mult), [ps, g1b], [tm])
                    dve(lambda: nc.vector.tensor_tensor(out=xt[:, nh * 512:(nh + 1) * 512], in0=xt[:, nh * 512:(nh + 1) * 512], in1=tm[:], op=ALU.add), [xt, tm], [xt])
                c.dma("act", dst.t[tt * 128:(tt + 1) * 128, :], xt[:], reads=[xt], writes=[dst])
        c.pop()

    def phase_moe(l, src, dst, final):
        phase_norm(l, 1, src)
        c.push()
        wsel = c.sbuf("wsel", [128, NTT, NEXP], F32)
        c.push()
        Wr = c.sbuf("Wr", [128, KC, NEXP], BF16)
        c.dma("pool", Wr[:], w_router.t[l].rearrange("(k p) e -> p k e", p=128), reads=[w_router], writes=[Wr])
        brr = c.sbuf("brr", [128, NEXP], F32)
        c.dma("sp", brr[:], b_router.t[l:l + 1, :].to_broadcast([128, NEXP]), reads=[b_router], writes=[brr])
        AFs = c.sbuf("AFs", [128, NTT, NEXP], F32)
        xbp = Pool(c, "rxb", [128, KC, 512], BF16, 2)
        smp = Pool(c, "rsm", [128, 2], F32, 4)
        lgp = Pool(c, "rlg", [128, NEXP], F32, 4)
        for blk in range(NBLK):
            xb = xbp.next()
            load_xmblk(xb, blk)
            for t4 in range(4):
                tt = blk * 4 + t4
                ps = PSF.next()
                mm_group(ps, ps[:, 0:NEXP], [(xb[:, kc, t4 * 128:(t4 + 1) * 128], Wr[:, kc, :]) for kc in range(KC)], [xb, Wr])
                lg = lgp.next(); sm = smp.next()
                dve(lambda: nc.vector.tensor_tensor(out=lg[:], in0=ps[:, 0:NEXP], in1=brr[:], op=ALU.add), [ps, brr], [lg])
                act(lambda: nc.scalar.activation(out=lg[:], in_=lg[:], func=AF.Exp, accum_out=sm[:, 0:1]), [lg], [lg, sm])
                dve(lambda: nc.vector.reciprocal(out=sm[:, 1:2], in_=sm[:, 0:1]), [sm], [sm])
                dve(lambda: nc.vector.tensor_scalar(out=AFs[:, tt, :], in0=lg[:], scalar1=sm[:, 1:2], scalar2=None, op0=ALU.mult), [lg, sm], [AFs])
        c.dma("act", AFF.t.rearrange("(t p) e -> p t e", p=128), AFs[:], reads=[AFs], writes=[AFF])
        c.allgather(AFF, AFFALL, GROUPS)
        NF = 4 * Tn // 128
        AA = c.sbuf("AA", [128, NF, NEXP], F32)
        c.dma("sp", AA[:], AFFALL.t.rearrange("(p f) e -> p f e", p=128), reads=[AFFALL], writes=[AA])
        cmpT = c.sbuf("cmpT", [128, NF, NEXP], F32)
        lo = c.sbuf("blo", [128, NEXP], F32); hi = c.sbuf("bhi", [128, NEXP], F32); mid = c.sbuf("bmid", [128, NEXP], F32)
        cnt = c.sbuf("bcnt", [128, NEXP], F32); ge = c.sbuf("bge", [128, NEXP], F32); d1 = c.sbuf("bd1", [128, NEXP], F32)
        dve(lambda: nc.vector.memset(lo[:], 0.0), [], [lo])
        dve(lambda: nc.vector.memset(hi[:], 1.0), [], [hi])
        for it in range(30):
            dve(lambda: nc.vector.tensor_tensor(out=mid[:], in0=lo[:], in1=hi[:], op=ALU.add), [lo, hi], [mid])
            dve(lambda: nc.vector.tensor_scalar(out=mid[:], in0=mid[:], scalar1=0.5, scalar2=None, op0=ALU.mult), [mid], [mid])
            dve(lambda: nc.vector.tensor_tensor(out=cmpT[:], in0=AA[:], in1=mid[:].unsqueeze(1).to_broadcast([128, NF, NEXP]), op=ALU.is_gt), [AA, mid], [cmpT])
            dve(lambda: nc.vector.tensor_reduce(out=cnt[:], in_=cmpT[:].rearrange("p f e -> p e f"), axis=AX.X, op=ALU.add), [cmpT], [cnt])
            ps = PSF.next()
            c.op("pe", lambda: nc.tensor.matmul(ps[:, 0:NEXP], lhsT=ones_f[:], rhs=cnt[:], start=True, stop=True), [ones_f, cnt], [ps])
            dve(lambda: nc.vector.tensor_scalar(out=ge[:], in0=ps[:, 0:NEXP], scalar1=float(cap) - 0.5, scalar2=None, op0=ALU.is_ge), [ps], [ge])
            dve(lambda: nc.vector.tensor_tensor(out=d1[:], in0=mid[:], in1=lo[:], op=ALU.subtract), [mid, lo], [d1])
            dve(lambda: nc.vector.tensor_tensor(out=d1[:], in0=d1[:], in1=ge[:], op=ALU.mult), [d1, ge], [d1])
            dve(lambda: nc.vector.tensor_tensor(out=lo[:], in0=lo[:], in1=d1[:], op=ALU.add), [lo, d1], [lo])
            dve(lambda: nc.vector.tensor_tensor(out=d1[:], in0=hi[:], in1=mid[:], op=ALU.subtract), [hi, mid], [d1])
            dve(lambda: nc.vector.tensor_tensor(out=d1[:], in0=d1[:], in1=ge[:], op=ALU.mult), [d1, ge], [d1])
            dve(lambda: nc.vector.tensor_tensor(out=hi[:], in0=mid[:], in1=d1[:], op=ALU.add), [mid, d1], [hi])
        dve(lambda: nc.vector.tensor_tensor(out=wsel[:], in0=AFs[:], in1=lo[:].unsqueeze(1).to_broadcast([128, NTT, NEXP]), op=ALU.is_gt), [AFs, lo], [wsel])
        dve(lambda: nc.vector.tensor_tensor(out=wsel[:], in0=wsel[:], in1=AFs[:], op=ALU.mult), [wsel, AFs], [wsel])
        c.pop()
        if "WSEL" in dbg:
            o = T(nc.dram_tensor("dbg_WSEL%d" % l, [128, NTT * NEXP], F32, kind="ExternalOutput").ap(), Tok("dbgw"))
            c.dma("sp", o.t, wsel[:].rearrange("p t e -> p (t e)"), reads=[wsel], writes=[o])
            dbg_out["WSEL%d" % l] = o
        TB = min(2048, Tn)
        NTB = Tn // TB
        yacc = c.sbuf("yacc", [128, TB // 128, D], F32)
        xmb = c.sbuf("xmb", [128, KC, TB], BF16)
        wgp = Pool(c, "ewg", [128, KC, 512], BF16, 2)
        wup = Pool(c, "ewu", [128, KC, 512], BF16, 2)
        wdp = Pool(c, "ewd", [128, 4, D], BF16, 2)
        htp = Pool(c, "eh", [128, 4, 512], BF16, 2)
        slp = Pool(c, "esl", [128, 512], F32, 3)
        xtp = Pool(c, "ex", [128, D], F32, 2)
        g2b = c.sbuf("g2b", [128, D], F32)
        fwb = c.sbuf("fwb", [128, D], F32)
        junk = c.sbuf("ejunk", [128, D], BF16)
        ssp = Pool(c, "ess", [128, 2], F32, 4)
        if final:
            c.dma("sp", fwb[:], final_norm_w.t[0:1, :].to_broadcast([128, D]), reads=[final_norm_w], writes=[fwb])
        for tb in range(NTB):
            c.dma("sp", xmb[:], XMT.t[:, :, tb * TB:(tb + 1) * TB].rearrange("k p t -> p k t"), reads=[XMT], writes=[xmb])
            dve(lambda: nc.vector.memset(yacc[:], 0.0), [], [yacc])
            for e in range(NEXP):
                for fq in range(DEXP // 512):
                    wg = wgp.next(); wu = wup.next(); wd = wdp.next()
                    idx = (l * NEXP + e) * NFQ + fq
                    c.dma("sp", wg[:].rearrange("p k n -> p (k n)"), EGb.t[idx], reads=[EGb], writes=[wg])
                    c.dma("sp", wu[:].rearrange("p k n -> p (k n)"), EUb.t[idx], reads=[EUb], writes=[wu])
                    c.dma("sp", wd[:].rearrange("p k n -> p (k n)"), EDb.t[idx], reads=[EDb], writes=[wd])
                    for sb in range(TB // 512):
                        ht = htp.next()
                        for fi in range(4):
                            psg = PSF.next()
                            mm_group(psg, psg[:], [(wg[:, kc, fi * 128:(fi + 1) * 128], xmb[:, kc, sb * 512:(sb + 1) * 512]) for kc in range(KC)], [wg, xmb])
                            psu = PSF.next()
                            mm_group(psu, psu[:], [(wu[:, kc, fi * 128:(fi + 1) * 128], xmb[:, kc, sb * 512:(sb + 1) * 512]) for kc in range(KC)], [wu, xmb])
                            sl = slp.next()
                            act(lambda: nc.scalar.activation(out=sl[:], in_=psg[:], func=AF.Silu), [psg], [sl])
                            dve(lambda: nc.vector.tensor_tensor(out=ht[:, fi, :], in0=psu[:], in1=sl[:], op=ALU.mult), [psu, sl], [ht])
                        for t4 in range(4):
                            ttl = sb * 4 + t4
                            ttg = tb * (TB // 128) + ttl
                            for nh in range(2):
                                ps = PSF.next()
                                mm_group(ps, ps[:], [(ht[:, fi, t4 * 128:(t4 + 1) * 128], wd[:, fi, nh * 512:(nh + 1) * 512]) for fi in range(4)], [ht, wd])
                                dve(lambda: nc.vector.scalar_tensor_tensor(out=yacc[:, ttl, nh * 512:(nh + 1) * 512], in0=ps[:], scalar=wsel[:, ttg, e:e + 1],
                                                                           in1=yacc[:, ttl, nh * 512:(nh + 1) * 512], op0=ALU.mult, op1=ALU.add), [ps, wsel, yacc], [yacc])
            cur_seg = -1
            for ttl in range(TB // 128):
                ttg = tb * (TB // 128) + ttl
                seg = seg_of_tt(ttg)
                if seg != cur_seg:
                    c.dma("sp", g2b[:], MOD.t[l, seg:seg + 1, 5 * D:6 * D].to_broadcast([128, D]), reads=[MOD], writes=[g2b])
                    cur_seg = seg
                xt = xtp.next()
                c.dma("sp", xt[:], src.t[ttg * 128:(ttg + 1) * 128, :], reads=[src], writes=[xt])
                dve(lambda: nc.vector.tensor_tensor(out=yacc[:, ttl, :], in0=yacc[:, ttl, :], in1=g2b[:], op=ALU.mult), [yacc, g2b], [yacc])
                dve(lambda: nc.vector.tensor_tensor(out=xt[:], in0=xt[:], in1=yacc[:, ttl, :], op=ALU.add), [xt, yacc], [xt])
                if final:
                    ss = ssp.next()
                    act(lambda: nc.scalar.activation(out=junk[:], in_=xt[:], func=AF.Square, accum_out=ss[:, 0:1]), [xt], [junk, ss])
                    act(lambda: nc.scalar.activation(out=ss[:, 1:2], in_=ss[:, 0:1], func=AF.Sqrt, bias=EPS, scale=1.0 / D), [ss], [ss])
                    dve(lambda: nc.vector.reciprocal(out=ss[:, 1:2], in_=ss[:, 1:2]), [ss], [ss])
                    dve(lambda: nc.vector.scalar_tensor_tensor(out=xt[:], in0=xt[:], scalar=ss[:, 1:2], in1=fwb[:], op0=ALU.mult, op1=ALU.mult), [xt, ss, fwb], [xt])
                c.dma("act", dst.t[ttg * 128:(ttg + 1) * 128, :], xt[:], reads=[xt], writes=[dst])
        c.pop()

    src = x_in
    done = False
    for l in range(2):
        phase_norm(l, 0, src)
        if l == 0:
            dbg_dump("XMT", XMT, [KC, 128, Tn], BF16)
        if stop_after == "normA":
            done = True; break
        c.push()
        GA = c.sbuf("GA", [128, NTT, 16], F32)
        phase_mlstm_proj(l, GA)
        if l == 0:
            dbg_dump("QKT", QKT, [16, 128, Tn], BF16)
            dbg_dump("KVO", KVO, [Tn, 3 * D], BF16)
        if stop_after == "mproj":
            c.pop(); done = True; break
        phase_mlstm_scan(l, GA)
        c.pop()
        if l == 0:
            dbg_dump("HF", HF, [Tn, D], F32)
            dbg_dump("HAT", HT[0], [KC, 128, Tn], BF16)
        if stop_after == "mscan":
            done = True; break
        phase_na_proj(l)
        phase_na_attn(l)
        if l == 0:
            dbg_dump("HBT", HT[1], [KC, 128, Tn], BF16)
        if stop_after == "na":
            done = True; break
        phase_lru_proj(l)
        phase_lru_scan(l)
        if l == 0:
            dbg_dump("HCT", HT[2], [KC, 128, Tn], BF16)
        if stop_after == "lru":
            done = True; break
        phase_merge(l, src, X[0])
        if l == 0:
            dbg_dump("X0", X[0], [Tn, D], F32)
        if stop_after == "merge":
            done = True; break
        last = (l == 1)
        phase_moe(l, X[0], y_out if last else X[1], last)
        if l == 0:
            dbg_dump("X1", X[1], [Tn, D], F32)
        if stop_after == "moe":
            done = True; break
        src = X[1]

    if done:
        c.push()
        tp = Pool(c, "cpy", [128, D], F32, 2)
        for tt in range(NTT):
            t = tp.next()
            c.dma("sp", t[:], x_in.t[tt * 128:(tt + 1) * 128, :], reads=[x_in], writes=[t])
            c.dma("act", y_out.t[tt * 128:(tt + 1) * 128, :], t[:], reads=[t], writes=[y_out])
        c.pop()
    c.close()
    return nc, list(dbg_out.keys())


def _rv_table(rows, prompt):
    rv = np.zeros((128, rows, 8), np.float32)
    rseg = rows // NSEG
    for R in range(rows):
        cs = min(max((R - 7) // 2, 0), rows // 2 - 8)
        if prompt:
            rs = min(max(R - 4, 0), rows - 8)
        else:
            s, r = divmod(R, rseg)
            rs = s * rseg + min(max(r - 4, 0), rseg - 8)
        for sl in range(8):
            for rr in range(2):
                kr = 2 * (cs + sl) + rr
                if rs <= kr <= rs + 7:
                    rv[rr * 64:(rr + 1) * 64, R, sl] = 1.0
    return rv.reshape(128, rows * 8)


def _bias_table(rpb):
    q = np.arange(64)
    kc = np.arange(64)
    cs = np.clip(q - 8, 0, 48)
    col_in = (kc[None, :] >= cs[:, None]) & (kc[None, :] < cs[:, None] + 16)
    dc = np.clip(kc[None, :] - q[:, None], -15, 15) + 15
    out = np.full((2, 128, 16, 16, 64), NEGM, np.float32)
    for rr in range(2):
        for x in range(16):
            di = x - 1 + rr
            if di < 0 or di > 14:
                continue
            g = rpb[:, :, di, :][:, :, dc]
            g = np.where(col_in[None, None], g, NEGM)
            out[:, rr * 64:(rr + 1) * 64, :, x, :] = np.transpose(g, (0, 3, 1, 2))
    return np.ascontiguousarray(out.reshape(2, 128, 16 * 16 * 64))


def _lru_bd(wa, wx):
    out = np.zeros((2, 4, 8, 128, 128), np.float32)
    for l in range(2):
        for d in range(2):
            for gi, w in enumerate((wa, wx)):
                for ft in range(8):
                    for b in range(2):
                        out[l, d * 2 + gi, ft, b * 64:(b + 1) * 64, b * 64:(b + 1) * 64] = w[l, d, ft * 2 + b]
    return out


def prep_inputs(inp, Tn, with_moe=True):
    f = lambda a: np.ascontiguousarray(np.asarray(a, np.float32))
    xp = f(inp["x_prompt"]); xs = f(inp["x_sample"]); cp = f(inp["c_prompt"]); cs_ = f(inp["c_sample"])
    rows = Tn // 64
    names = ["norm1_w", "norm2_w", "w_mod", "b_mod", "w_in", "b_in", "mlstm_norm_w", "conv_w", "conv_b",
             "lru_ba", "lru_bx", "lru_L", "w_br_a", "w_br_b", "w_br_c", "w_out", "w_router", "b_router"]
    if with_moe:
        names += ["w_gate_e", "w_up_e", "w_down_e"]
    shared = {k: f(inp[k]) for k in names}
    shared["final_norm_w"] = f(inp["final_norm_w"]).reshape(1, D)
    shared["btab"] = _bias_table(f(inp["na_rpb"]))
    shared["lru_bd"] = _lru_bd(f(inp["lru_wa"]), f(inp["lru_wx"]))
    rvp = _rv_table(rows, True); rvs = _rv_table(rows, False)
    maps = []
    nps = xs.shape[0] // 4
    for i in range(8):
        if i < 4:
            x = xp[i].reshape(Tn, D)
            c4 = np.repeat(cp[i:i + 1], NSEG, axis=0)
            keep = np.ones((128, 1), np.float32); rv = rvp
        else:
            j = i - 4
            x = xs[j * nps:(j + 1) * nps].reshape(Tn, D)
            c4 = cs_[j * nps:(j + 1) * nps]
            keep = np.zeros((128, 1), np.float32); rv = rvs
        c4T = np.ascontiguousarray(c4.reshape(NSEG, KC, 128).transpose(2, 1, 0))
        m = dict(shared)
        m.update(x=np.ascontiguousarray(x), c4T=c4T, keep=keep, rv=rv)

        maps.append(m)
    return maps


_CACHE = {}


def run(inp, stop_after=None, dbg=()):
    B, S, _ = inp["x_prompt"].shape
    Tn = S
    assert inp["x_sample"].shape[0] * inp["x_sample"].shape[1] == 4 * Tn
    cap = 2 * (B * S) // NEXP
    dexp = inp["w_gate_e"].shape[-1]
    key = (Tn, cap, stop_after, tuple(dbg), dexp)
    if key not in _CACHE:
        _CACHE[key] = build(Tn, cap, stop_after, dbg, DEXP=dexp)
    nc, dnames = _CACHE[key]
    maps = prep_inputs(inp, Tn, with_moe=(stop_after is None or stop_after in ("moe",)))
    res = run_bass_kernel_spmd(nc, maps, core_ids=list(range(8)))
    ys = [r["y"] for r in res.results]
    yp = np.stack(ys[:4], 0).reshape(inp["x_prompt"].shape).astype(np.float32)
    ysm = np.concatenate(ys[4:], 0).reshape(inp["x_sample"].shape).astype(np.float32)
    return (yp, ysm), res.results


def kernel(**inputs):
    out, _ = run(inputs)
    return out
```
